# Optimizing a Trainium2 kernel written in Bass

```python
import functools
import jax, jax.numpy as jnp
from jax import lax
import numpy as np

D_MODEL = 1024
BATCH = 4
SEQ = 4096
DEPTH = 2

GRID_W = 64
CTX_LEN = 256
HEAD_DIM = 64
N_CONV_GROUPS = D_MODEL // 256
N_RET_HEADS = (D_MODEL // HEAD_DIM - N_CONV_GROUPS) // 2
N_NA_HEADS = D_MODEL // HEAD_DIM - N_CONV_GROUPS - N_RET_HEADS
D_RET = N_RET_HEADS * HEAD_DIM
D_CONV = N_CONV_GROUPS * HEAD_DIM
D_NA = N_NA_HEADS * HEAD_DIM
D_MIX = D_RET + D_CONV + D_NA
D_IN_PROJ = 4 * D_RET + 3 * D_CONV + 3 * D_NA
RET_CHUNK = 128
CONV_WIDTH = 3
NA_ROWS = 8
NA_COLS = 16
D_FF = 2816
N_EXPERTS = 8
TOP_K = 2
D_FF_EXPERT = 2816
N_DENSE = (DEPTH + 1) // 2
N_MOE = DEPTH // 2
ROPE_BASE = 10000.0
NORM_EPS = 1e-6

kernel_name = 'hybrid_retention_shortconv_natten_moe_dit'


def rms_norm(x, g):
    xf = x.astype(jnp.float32)
    y = xf * lax.rsqrt(jnp.mean(xf * xf, axis=-1, keepdims=True) + NORM_EPS)
    return (y * g.astype(jnp.float32)).astype(x.dtype)


def modulate(h, shift, scale):
    return h * (1.0 + scale) + shift


def split_heads(t, n_heads):
    b, n, _ = t.shape
    return t.reshape(b, n, n_heads, HEAD_DIM).transpose(0, 2, 1, 3)


def merge_heads(t):
    b, h, n, d = t.shape
    return t.transpose(0, 2, 1, 3).reshape(b, n, h * d)


def flip_seq(t):
    return jnp.flip(t, axis=2)


def split_in_proj(t):
    sizes = (D_RET,) * 4 + (D_CONV,) * 3 + (D_NA,) * 3
    points = np.cumsum(sizes)[:-1].tolist()
    return jnp.split(t, points, axis=-1)


def axial_rope_tables(n_tok):
    t = jnp.arange(n_tok, dtype=jnp.int32)
    row = (t // GRID_W).astype(jnp.float32)
    col = (t % GRID_W).astype(jnp.float32)
    n_freq = HEAD_DIM // 4
    inv_freq = ROPE_BASE ** (-jnp.arange(n_freq, dtype=jnp.float32) / n_freq)
    ang_r = row[:, None] * inv_freq
    ang_c = col[:, None] * inv_freq
    return (jnp.cos(ang_r), jnp.sin(ang_r), jnp.cos(ang_c), jnp.sin(ang_c))


def _rotate(x, cos, sin):
    x1, x2 = jnp.split(x, 2, axis=-1)
    return jnp.concatenate([x1 * cos - x2 * sin, x1 * sin + x2 * cos], axis=-1)


def apply_axial_rope(x, rope):
    cos_r, sin_r, cos_c, sin_c = (r.astype(x.dtype) for r in rope)
    xr, xc = jnp.split(x, 2, axis=-1)
    return jnp.concatenate([_rotate(xr, cos_r, sin_r), _rotate(xc, cos_c, sin_c)], axis=-1)


def retention_final_state(k, v, log_gamma):
    n_tok = k.shape[2]
    w = jnp.exp(log_gamma[:, None] * (n_tok - 1.0 - jnp.arange(n_tok, dtype=jnp.float32)))
    return jnp.einsum('bhnd,bhne->bhde', k.astype(jnp.float32) * w[:, :, None], v.astype(jnp.float32))


def retention_chunkwise(q, k, v, log_gamma, s0):
    b, h, n_tok, dh = q.shape
    n = n_tok // RET_CHUNK
    qc, kc, vc = (t.astype(jnp.float32).reshape(b, h, n, RET_CHUNK, dh) for t in (q, k, v))
    pos = jnp.arange(RET_CHUNK, dtype=jnp.float32)
    lg = log_gamma[:, None]
    diff = pos[:, None] - pos[None, :]
    intra_decay = jnp.where(diff >= 0, jnp.exp(lg[:, :, None] * jnp.maximum(diff, 0.0)), 0.0)
    q_decay = jnp.exp(lg * (pos + 1.0))
    k_decay = jnp.exp(lg * (RET_CHUNK - 1.0 - pos))
    chunk_decay = jnp.exp(log_gamma * RET_CHUNK)[None, :, None, None]
    scores = jnp.einsum('bhnid,bhnjd->bhnij', qc, kc) * intra_decay[:, None]
    o_intra = jnp.einsum('bhnij,bhnjd->bhnid', scores, vc)
    u = jnp.einsum('bhnjd,bhnje->nbhde', kc * k_decay[:, None, :, None], vc)

    def step(state, u_n):
        return chunk_decay * state + u_n, state

    _, s_before = lax.scan(step, s0, u)
    o_inter = jnp.einsum('bhnid,nbhde->bhnie', qc * q_decay[:, None, :, None], s_before)
    return (o_intra + o_inter).reshape(b, h, n_tok, dh)


def bidir_retention(q, k, v, log_gamma, s_fwd0, s_bwd0):
    o_f = retention_chunkwise(q, k, v, log_gamma[0], s_fwd0)
    o_b = retention_chunkwise(flip_seq(q), flip_seq(k), flip_seq(v), log_gamma[1], s_bwd0)
    return o_f + flip_seq(o_b)


def retention_output(o, gate, gn_g):
    mu = jnp.mean(o, axis=-1, keepdims=True)
    var = jnp.mean(jnp.square(o - mu), axis=-1, keepdims=True)
    o = merge_heads((o - mu) * lax.rsqrt(var + NORM_EPS)) * gn_g.astype(jnp.float32)
    return (jax.nn.silu(gate.astype(jnp.float32)) * o).astype(gate.dtype)


def short_gated_conv(b_gate, c_gate, x_in, conv_w):
    u = c_gate * x_in
    w = conv_w[:, None, :].astype(u.dtype)
    y = lax.conv_general_dilated(u, w, window_strides=(1,), padding=((CONV_WIDTH // 2, CONV_WIDTH // 2),),
                                 dimension_numbers=('NWC', 'WIO', 'NWC'), feature_group_count=u.shape[-1])
    return b_gate * y


def neighbourhood_attention(q, k, v, k_ctx, v_ctx, rpb):
    b, h, n_tok, dh = q.shape
    rows = n_tok // GRID_W
    kr = min(NA_ROWS, rows)
    scale = dh ** -0.5
    r = jnp.arange(rows)
    cidx = jnp.arange(GRID_W)
    r0 = jnp.clip(r - kr // 2, 0, rows - kr)
    key_rows = r0[:, None] + jnp.arange(kr)[None, :]
    c0 = jnp.clip(cidx - NA_COLS // 2, 0, GRID_W - NA_COLS)
    col_in = (cidx[None, :] >= c0[:, None]) & (cidx[None, :] < c0[:, None] + NA_COLS)
    qg = q.reshape(b, h, rows, GRID_W, dh)
    kg = k.reshape(b, h, rows, GRID_W, dh)[:, :, key_rows]
    vg = v.reshape(b, h, rows, GRID_W, dh)[:, :, key_rows]
    s_loc = jnp.einsum('bhrqd,bhrikd->bhrqik', qg, kg).astype(jnp.float32) * scale
    dr = key_rows - r[:, None] + (NA_ROWS - 1)
    dc = jnp.clip(cidx[None, :] - cidx[:, None], -(NA_COLS - 1), NA_COLS - 1) + (NA_COLS - 1)
    bias = rpb[:, dr[:, None, :, None], dc[None, :, None, :]].astype(jnp.float32)
    s_loc = jnp.where(col_in[None, None, None, :, None, :], s_loc + bias[None], -jnp.inf)
    s_loc = s_loc.reshape(b, h, rows, GRID_W, kr * GRID_W)
    s_ctx = jnp.einsum('bhrqd,bhld->bhrql', qg, k_ctx).astype(jnp.float32) * scale
    p = jax.nn.softmax(jnp.concatenate([s_loc, s_ctx], axis=-1), axis=-1)
    p_loc = p[..., :kr * GRID_W].reshape(b, h, rows, GRID_W, kr, GRID_W).astype(v.dtype)
    p_ctx = p[..., kr * GRID_W:].astype(v.dtype)
    o = jnp.einsum('bhrqik,bhrikd->bhrqd', p_loc, vg) + jnp.einsum('bhrql,bhld->bhrqd', p_ctx, v_ctx)
    return o.reshape(b, h, n_tok, dh)


def context_attention(q, k, v):
    s = jnp.einsum('bhqd,bhkd->bhqk', q, k).astype(jnp.float32) * (q.shape[-1] ** -0.5)
    p = jax.nn.softmax(s, axis=-1).astype(v.dtype)
    return jnp.einsum('bhqk,bhkd->bhqd', p, v)


def mixer_block(a_lat, a_ctx, w_in, w_out, ret_decay_logit, ret_gn_g, conv_w, na_rpb, rope, with_ctx_out):
    rq, rk, rv, rg, cb, cc, cx, nq, nk, nv = split_in_proj(a_lat @ w_in)
    crq, crk, crv, crg, ccb, ccc, ccx, cnq, cnk, cnv = split_in_proj(a_ctx @ w_in)
    log_gamma = jax.nn.log_sigmoid(ret_decay_logit.astype(jnp.float32))
    k_scale = HEAD_DIM ** -0.5
    rk_c = split_heads(crk, N_RET_HEADS) * k_scale
    rv_c = split_heads(crv, N_RET_HEADS)
    s_fwd = retention_final_state(rk_c, rv_c, log_gamma[0])
    s_bwd = retention_final_state(flip_seq(rk_c), flip_seq(rv_c), log_gamma[1])
    nk_c = split_heads(cnk, N_NA_HEADS)
    nv_c = split_heads(cnv, N_NA_HEADS)
    rq_l = apply_axial_rope(split_heads(rq, N_RET_HEADS), rope)
    rk_l = apply_axial_rope(split_heads(rk, N_RET_HEADS), rope) * k_scale
    rv_l = split_heads(rv, N_RET_HEADS)
    y_ret = retention_output(bidir_retention(rq_l, rk_l, rv_l, log_gamma, s_fwd, s_bwd), rg, ret_gn_g)
    y_conv = short_gated_conv(cb, cc, cx, conv_w)
    y_na = merge_heads(neighbourhood_attention(split_heads(nq, N_NA_HEADS), split_heads(nk, N_NA_HEADS),
                                               split_heads(nv, N_NA_HEADS), nk_c, nv_c, na_rpb))
    y_lat = jnp.concatenate([y_ret, y_conv, y_na], axis=-1) @ w_out
    if not with_ctx_out:
        return y_lat, None
    zero = jnp.zeros_like(s_fwd)
    y_ret_c = retention_output(bidir_retention(split_heads(crq, N_RET_HEADS), rk_c, rv_c, log_gamma, zero, zero),
                               crg, ret_gn_g)
    y_conv_c = short_gated_conv(ccb, ccc, ccx, conv_w)
    y_na_c = merge_heads(context_attention(split_heads(cnq, N_NA_HEADS), nk_c, nv_c))
    y_ctx = jnp.concatenate([y_ret_c, y_conv_c, y_na_c], axis=-1) @ w_out
    return y_lat, y_ctx


def swiglu(h, w_in, w_out):
    gate, up = jnp.split(h @ w_in, 2, axis=-1)
    return (jax.nn.silu(gate) * up) @ w_out


def moe_swiglu(h, router_w, router_b, w_in, w_out):
    logits = (h @ router_w).astype(jnp.float32) + router_b.astype(jnp.float32)
    top_v, top_i = lax.top_k(logits, TOP_K)
    wts = jax.nn.softmax(top_v, axis=-1)
    gates = jnp.sum(jax.nn.one_hot(top_i, N_EXPERTS, dtype=jnp.float32) * wts[..., None], axis=-2)
    y = jnp.zeros_like(h)
    for e in range(N_EXPERTS):
        y = y + gates[..., e:e + 1].astype(h.dtype) * swiglu(h, w_in[e], w_out[e])
    return y


def setup_inputs(seed: int = 0) -> dict:
    key = jax.random.key(seed)
    ks = jax.random.split(key, 24)
    f32 = jnp.float32

    def nrm(k, shape, scale):
        return scale * jax.random.normal(k, shape, f32)

    base_logit = jnp.log(2.0 ** (5.0 + jnp.arange(N_RET_HEADS, dtype=f32)) - 1.0)
    return {
        'x': nrm(ks[0], (BATCH, SEQ, D_MODEL), 1.0),
        'c': nrm(ks[1], (BATCH, D_MODEL), 1.0),
        'ctx': nrm(ks[2], (BATCH, CTX_LEN, D_MODEL), 1.0),
        'c_ctx': nrm(ks[3], (D_MODEL,), 1.0),
        'ada_w': nrm(ks[4], (DEPTH, D_MODEL, 6 * D_MODEL), 0.5 * D_MODEL ** -0.5),
        'ada_b': nrm(ks[5], (DEPTH, 6 * D_MODEL), 0.02),
        'norm1_g': 1.0 + nrm(ks[6], (DEPTH, D_MODEL), 0.02),
        'norm2_g': 1.0 + nrm(ks[7], (DEPTH, D_MODEL), 0.02),
        'w_in': nrm(ks[8], (DEPTH, D_MODEL, D_IN_PROJ), D_MODEL ** -0.5),
        'w_out': nrm(ks[9], (DEPTH, D_MIX, D_MODEL), D_MIX ** -0.5),
        'ret_decay_logit': base_logit + nrm(ks[10], (DEPTH, 2, N_RET_HEADS), 0.1),
        'ret_gn_g': 1.0 + nrm(ks[11], (DEPTH, D_RET), 0.02),
        'conv_w': nrm(ks[12], (DEPTH, CONV_WIDTH, D_CONV), CONV_WIDTH ** -0.5),
        'na_rpb': nrm(ks[13], (DEPTH, N_NA_HEADS, 2 * NA_ROWS - 1, 2 * NA_COLS - 1), 0.1),
        'ffn_w_in': nrm(ks[14], (N_DENSE, D_MODEL, 2 * D_FF), D_MODEL ** -0.5),
        'ffn_w_out': nrm(ks[15], (N_DENSE, D_FF, D_MODEL), D_FF ** -0.5),
        'moe_router_w': nrm(ks[16], (N_MOE, D_MODEL, N_EXPERTS), D_MODEL ** -0.5),
        'moe_router_b': nrm(ks[17], (N_MOE, N_EXPERTS), 0.01),
        'moe_w_in': nrm(ks[18], (N_MOE, N_EXPERTS, D_MODEL, 2 * D_FF_EXPERT), D_MODEL ** -0.5),
        'moe_w_out': nrm(ks[19], (N_MOE, N_EXPERTS, D_FF_EXPERT, D_MODEL), D_FF_EXPERT ** -0.5),
        'final_g': 1.0 + nrm(ks[20], (D_MODEL,), 0.02),
    }


def reference(x, c, ctx, c_ctx, ada_w, ada_b, norm1_g, norm2_g, w_in, w_out, ret_decay_logit, ret_gn_g,
              conv_w, na_rpb, ffn_w_in, ffn_w_out, moe_router_w, moe_router_b, moe_w_in, moe_w_out, final_g):
    rope = axial_rope_tables(x.shape[1])
    c_act = jax.nn.silu(c)
    cctx_act = jax.nn.silu(c_ctx)
    h, hc = x, ctx
    for layer in range(DEPTH):
        update_ctx = layer < DEPTH - 1
        sh1, sc1, g1, sh2, sc2, g2 = jnp.split((c_act @ ada_w[layer] + ada_b[layer])[:, None, :], 6, axis=-1)
        csh1, csc1, cg1, csh2, csc2, cg2 = jnp.split(cctx_act @ ada_w[layer] + ada_b[layer], 6, axis=-1)
        a_lat = modulate(rms_norm(h, norm1_g[layer]), sh1, sc1)
        a_ctx = modulate(rms_norm(hc, norm1_g[layer]), csh1, csc1)
        y_lat, y_ctx = mixer_block(a_lat, a_ctx, w_in[layer], w_out[layer], ret_decay_logit[layer], ret_gn_g[layer],
                                   conv_w[layer], na_rpb[layer], rope, update_ctx)
        h = h + g1 * y_lat
        if layer % 2 == 0:
            ffn = functools.partial(swiglu, w_in=ffn_w_in[layer // 2], w_out=ffn_w_out[layer // 2])
        else:
            e = layer // 2
            ffn = functools.partial(moe_swiglu, router_w=moe_router_w[e], router_b=moe_router_b[e],
                                    w_in=moe_w_in[e], w_out=moe_w_out[e])
        h = h + g2 * ffn(modulate(rms_norm(h, norm2_g[layer]), sh2, sc2))
        if update_ctx:
            hc = hc + cg1 * y_ctx
            hc = hc + cg2 * ffn(modulate(rms_norm(hc, norm2_g[layer]), csh2, csc2))
    return rms_norm(h, final_g)
```

```python
import numpy as np
import ml_dtypes
import concourse.bass as bass
import concourse.mybir as mybir
from concourse.bass_utils import run_bass_kernel_spmd
from contextlib import ExitStack

F32 = mybir.dt.float32
BF16 = mybir.dt.bfloat16
ALU = mybir.AluOpType
AF = mybir.ActivationFunctionType
AX = mybir.AxisListType

D = 1024
SEQ = 4096
CTX = 256
NT = 34
DIN = 3456
DFF = 2816
NEXP = 8
NEG = -30000.0
EPS = 1e-6
DEPTH = 2

SEM_LIM = 12000
import os as _os
_PE_EXEMPT = _os.environ.get("TRK_PE_EXEMPT", "1") == "1"
_PRUNE = _os.environ.get("TRK_PRUNE", "1") == "1"
_P1ST = _os.environ.get("P1_STORE_ENG", "sp")
DMA_K = 8


class Buf:
    __slots__ = ("name", "w", "r")

    def __init__(self, name):
        self.name = name
        self.w = None
        self.r = []


class Op:
    __slots__ = ("eng", "kind", "fn", "deps", "idx", "eidx", "need_sig", "sig", "slot", "target", "didx", "force")

    def __init__(self, eng, kind, fn):
        self.eng = eng
        self.kind = kind
        self.fn = fn
        self.deps = []
        self.need_sig = False
        self.sig = None
        self.slot = None
        self.target = None
        self.didx = None
        self.force = False


class Sched:
    ENGS = ("sp", "act", "pool", "pe", "dve")

    def __init__(self, nc):
        self.nc = nc
        self.ops = []
        self.per_eng = {e: [] for e in self.ENGS}
        self.ndma = {e: 0 for e in self.ENGS}
        self.bufs = {}

    def buf(self, name):
        b = self.bufs.get(name)
        if b is None:
            b = Buf(name)
            self.bufs[name] = b
        return b

    def add(self, eng, fn, reads=(), writes=(), kind="cmp", serial=False):
        op = Op(eng, kind, fn)
        op.idx = len(self.ops)
        op.eidx = len(self.per_eng[eng])
        op.force = serial
        if kind == "dma":
            op.didx = self.ndma[eng]
            self.ndma[eng] += 1
            op.slot = op.didx % DMA_K
            op.target = 16 * (op.didx // DMA_K + 1)
        deps = set()
        rl = [self.buf(b) if isinstance(b, str) else b for b in reads]
        wl = [self.buf(b) if isinstance(b, str) else b for b in writes]
        for b in rl:
            for w_ in (b.w or ()):
                deps.add(w_)
        for b in wl:
            for w_ in (b.w or ()):
                if not (kind == "dma" and self.ops[w_].kind == "dma"):
                    deps.add(w_)
            for r in b.r:
                deps.add(r)
        for b in rl:
            if kind == "cmp" and _PRUNE:
                b.r = [r for r in b.r if not (self.ops[r].kind == "cmp" and self.ops[r].eng == eng)]
            b.r.append(op.idx)
        for b in wl:
            if kind == "dma" and b.w and all(self.ops[w_].kind == "dma" for w_ in b.w) and not b.r:
                b.w = b.w + [op.idx]
            else:
                b.w = [op.idx]
            b.r = []
        if serial and self.per_eng[eng]:
            prev = self.per_eng[eng][-1]
            if prev.kind != "bar":
                deps.add(prev.idx)
        deps.discard(op.idx)
        op.deps = sorted(deps)
        self.ops.append(op)
        self.per_eng[eng].append(op)
        return op

    def dma(self, eng, out, in_, reads=(), writes=(), **kw):
        return self.add(eng, lambda e: e.dma_start(out=out, in_=in_, **kw), reads, writes, kind="dma")

    def barrier(self):
        deps = []
        for e in self.ENGS:
            lst = self.per_eng[e]
            last_c = None
            slots = {}
            for op in reversed(lst):
                if op.kind == "cmp" and last_c is None:
                    last_c = op
                elif op.kind == "dma" and op.slot not in slots:
                    slots[op.slot] = op
                if last_c is not None and len(slots) == DMA_K:
                    break
            if last_c is not None:
                deps.append(last_c.idx)
            deps.extend(o.idx for o in slots.values())
        for e in self.ENGS:
            op = Op(e, "bar", None)
            op.idx = len(self.ops)
            op.eidx = len(self.per_eng[e])
            op.deps = sorted(deps)
            self.ops.append(op)
            self.per_eng[e].append(op)

    def emit(self):
        nc = self.nc
        ops = self.ops
        need = {}
        for op in ops:
            lst = []
            for d in op.deps:
                dop = ops[d]
                if dop.kind == "bar":
                    continue
                if dop.kind == "dma":
                    lst.append(d)
                elif dop.eng == op.eng:
                    if op.kind != "cmp" or op.eng != "pe" or not _PE_EXEMPT or (op.force and (op.eidx - dop.eidx) <= 3):
                        lst.append(d)
                        dop.need_sig = True
                else:
                    lst.append(d)
                    dop.need_sig = True
            need[op.idx] = lst
        cnt = {e: 0 for e in self.ENGS}
        for e in self.ENGS:
            for op in self.per_eng[e]:
                if op.kind == "cmp" and op.need_sig:
                    cnt[e] += 1
                    op.sig = cnt[e]
        with ExitStack() as st:
            csem = {}
            for e in self.ENGS:
                n = cnt[e] // SEM_LIM + 1
                csem[e] = [st.enter_context(nc.semaphore(f"c_{e}_{i}")) for i in range(n)]
            dsem = {}
            for e in self.ENGS:
                if self.ndma[e]:
                    dsem[e] = [st.enter_context(nc.semaphore(f"d_{e}_{i}")) for i in range(DMA_K)]
            block = st.enter_context(nc.Block())

            def run_engine(e, eng):
                seen_c = {x: 0 for x in self.ENGS}
                seen_d = {}
                for op in self.per_eng[e]:
                    if op.kind == "dma" and op.didx >= DMA_K:
                        key = (e, op.slot)
                        tgt = op.target - 16
                        if seen_d.get(key, 0) < tgt:
                            eng.wait_ge(dsem[e][op.slot], tgt)
                            seen_d[key] = tgt
                    for d in need[op.idx]:
                        dop = ops[d]
                        if dop.kind == "dma":
                            key = (dop.eng, dop.slot)
                            if seen_d.get(key, 0) < dop.target:
                                eng.wait_ge(dsem[dop.eng][dop.slot], dop.target)
                                seen_d[key] = dop.target
                        else:
                            if seen_c[dop.eng] < dop.sig:
                                s = dop.sig - 1
                                if s // SEM_LIM > (seen_c[dop.eng] - 1) // SEM_LIM and seen_c[dop.eng] > 0:
                                    pass
                                eng.wait_ge(csem[dop.eng][s // SEM_LIM], s % SEM_LIM + 1)
                                seen_c[dop.eng] = dop.sig
                    if op.kind == "bar":
                        continue
                    ins = op.fn(eng)
                    if op.kind == "dma":
                        ins.then_inc(dsem[e][op.slot], 16)
                    elif op.sig is not None:
                        s = op.sig - 1
                        ins.then_inc(csem[e][s // SEM_LIM], 1)
                if self.ndma[e]:
                    for slot in range(DMA_K):
                        last = None
                        for op in reversed(self.per_eng[e]):
                            if op.kind == "dma" and op.slot == slot:
                                last = op
                                break
                        if last is not None and seen_d.get((e, slot), 0) < last.target:
                            eng.wait_ge(dsem[e][slot], last.target)

            if self.per_eng["sp"]:
                @block.sync
                def _(eng):
                    run_engine("sp", eng)
            if self.per_eng["act"]:
                @block.scalar
                def _(eng):
                    run_engine("act", eng)
            if self.per_eng["pool"]:
                @block.gpsimd
                def _(eng):
                    run_engine("pool", eng)
            if self.per_eng["pe"]:
                @block.tensor
                def _(eng):
                    run_engine("pe", eng)
            if self.per_eng["dve"]:
                @block.vector
                def _(eng):
                    run_engine("dve", eng)


class Arena:
    def __init__(self, ap, cap):
        self.ap = ap
        self.cap = cap
        self.off = 0

    def mark(self):
        return self.off

    def release(self, m):
        self.off = m

    def _alloc(self, words):
        o = self.off
        self.off += words
        assert self.off <= self.cap, f"arena overflow {self.off} > {self.cap}"
        return o

    def f32(self, n):
        o = self._alloc(n)
        return self.ap[:, o:o + n]

    def bf16(self, n):
        w = (n + 1) // 2
        o = self._alloc(w)
        return self.ap[:, o:o + w].bitcast(BF16)


ARENA_WORDS = 47600


def pbcast(ap2d_row, n):
    return bass.AP(ap2d_row.tensor, ap2d_row.offset, [[0, 128], [1, n]])


class Builder:
    def __init__(self, dbg=None, stop=None):
        self.dbg = dbg
        self.stop = stop
        self.nc = bass.Bass("TRN2", target_bir_lowering=False)
        self.S = Sched(self.nc)

    def mm(self, out, lhsT, rhs, start, stop, R, W, serial=False):
        self.S.add("pe", lambda e: e.matmul(out, lhsT, rhs, start=start, stop=stop), R, W, serial=serial)

    def tr(self, out, in_, ident, R, W):
        self.S.add("pe", lambda e: e.transpose(out, in_, ident), R, W)

    def act(self, out, in_, func, R, W, **kw):
        self.S.add("act", lambda e: e.activation(out, in_, func, **kw), R, W)

    def cp(self, eng, out, in_, R, W):
        if eng == "act":
            self.S.add("act", lambda e: e.copy(out, in_), R, W)
        else:
            self.S.add(eng, lambda e: e.tensor_copy(out, in_), R, W)

    def tt(self, eng, out, in0, in1, op, R, W):
        self.S.add(eng, lambda e: e.tensor_tensor(out, in0, in1, op), R, W)

    def ts(self, eng, out, in0, s1, s2, op0, op1, R, W):
        if s2 is None:
            self.S.add(eng, lambda e: e.tensor_scalar(out, in0, s1, None, op0), R, W)
        else:
            self.S.add(eng, lambda e: e.tensor_scalar(out, in0, s1, s2, op0, op1), R, W)

    def stt(self, eng, out, in0, sc, in1, op0, op1, R, W):
        self.S.add(eng, lambda e: e.scalar_tensor_tensor(out, in0, sc, in1, op0, op1), R, W)

    def sumsq(self, junk, x, acc, R, W):
        self.S.add("dve", lambda e: e.scalar_tensor_tensor(junk, x, 1.0, x, ALU.mult, ALU.mult, accum_out=acc), R, W)

    def red(self, out, in_, op, R, W):
        self.S.add("dve", lambda e: e.tensor_reduce(out, in_, AX.X, op), R, W)

    def memset(self, eng, ap, val, W):
        self.S.add(eng, lambda e: e.memset(ap, val), (), W)

    def ld(self, out, in_, R, W, eng="sp", **kw):
        self.S.dma(eng, out, in_, R, W, **kw)

    def build(self):
        nc = self.nc
        S = self.S

        def din(name, shape, dt=F32):
            return nc.dram_tensor(name, list(shape), dt, kind="ExternalInput").ap()

        def dscr(name, shape, dt):
            return nc.dram_tensor(name, list(shape), dt).ap()

        I = {}
        I["x"] = din("x", [SEQ, D]); I["ctx"] = din("ctx", [CTX, D]); I["cc"] = din("cc", [2, D])
        I["ada_w"] = din("ada_w", [2, D, 6 * D]); I["ada_b"] = din("ada_b", [2, 6 * D])
        I["norm1_g"] = din("norm1_g", [2, D]); I["norm2_g"] = din("norm2_g", [2, D])
        I["w_in"] = din("w_in", [2, D, DIN]); I["w_out"] = din("w_out", [2, D, D])
        I["rdl"] = din("ret_decay_logit", [2, 2, 6]); I["gng"] = din("ret_gn_g", [2, 384])
        I["conv_w"] = din("conv_w", [2, 3, 256]); I["rpb"] = din("na_rpb", [2, 6, 15, 31])
        I["ffn_w_in"] = din("ffn_w_in", [1, D, 2 * DFF]); I["ffn_w_out"] = din("ffn_w_out", [1, DFF, D])
        I["rw"] = din("moe_router_w", [1, D, NEXP]); I["rb"] = din("moe_router_b", [1, NEXP])
        I["moe_w_in"] = din("moe_w_in", [1, NEXP, D, 2 * DFF]); I["moe_w_out"] = din("moe_w_out", [1, NEXP, DFF, D])
        I["final_g"] = din("final_g", [D]); I["sel"] = din("sel", [128, 2])
        I["ident_bf"] = din("ident_bf", [128, 128], BF16); I["jrev_bf"] = din("jrev_bf", [128, 128], BF16)
        I["ident_f"] = din("ident_f", [128, 128]); I["jrev_f"] = din("jrev_f", [128, 128])
        I["rope_c"] = din("rope_c", [NT * 128, 64]); I["rope_s"] = din("rope_s", [NT * 128, 64])
        I["pos4"] = din("pos4", [128, 4]); I["iota12"] = din("iota12", [128, 2, 128])
        I["dtab"] = din("dtab", [128, 4, 128]); I["blockmask"] = din("blockmask", [128, 128])
        I["na_cmask"] = din("na_cmask", [128, 64])
        self.I = I
        self.out = nc.dram_tensor("out", [2048, D], F32, kind="ExternalOutput").ap()
        if self.dbg:
            self.dbg_outs = {n: nc.dram_tensor(n, list(sh), dt_, kind="ExternalOutput").ap() for (n, sh, dt_) in self.dbg[1]}

        self.H = dscr("H", [NT * 128, D], F32)
        self.H1 = dscr("H1", [NT * 128, D], F32)
        self.MOD = dscr("MOD", [2, 6 * D], F32)
        self.TM = dscr("TM", [NT * 128, 1408], BF16)
        self.UU = dscr("UU", [NT * 128 + 3, 256], BF16)
        self.NV = dscr("NV", [NT * 128, 384], BF16)
        self.QT = dscr("QT", [NT, 128, 384], BF16)
        self.KT = dscr("KT", [NT, 128, 384], BF16)
        self.NQT = dscr("NQT", [NT, 128, 384], BF16)
        self.NKT = dscr("NKT", [3, 128, NT * 128], BF16)
        self.ST = dscr("ST", [NT, 2, 128, 384], BF16)
        self.PZ = dscr("PZ", [90, 127], F32)
        self.BE = dscr("BE", [4, 128, 6 * 576], F32)
        if self.dbg:
            self.YC = dscr("YC", [NT * 128, D], BF16)

        with ExitStack() as st:
            arena_t = st.enter_context(nc.sbuf_tensor("arena", [128, ARENA_WORDS], F32))
            pA_t = st.enter_context(nc.psum_tensor("pA", [128, 6, 512], F32))
            pT_t = st.enter_context(nc.psum_tensor("pT", [128, 2, 1024], BF16))
            self.ar = Arena(arena_t[:, :], ARENA_WORDS)
            self.pA = [pA_t[:, i, :] for i in range(6)]
            self.pT = [pT_t[:, i, :] for i in range(2)]
            self.pAn = [f"pA{i}" for i in range(6)]
            self.pTn = [f"pT{i}" for i in range(2)]
            self.program()
            S.emit()
        return nc

    def program(self):
        S = self.S
        ar = self.ar
        I = self.I
        self.ident = ar.bf16(128); self.jrev = ar.bf16(128)
        self.ld(self.ident, I["ident_bf"], (), ["ident"]); self.ld(self.jrev, I["jrev_bf"], (), ["jrev"])
        self.cB = ar.bf16(16)
        m0 = ar.mark()
        cT = ar.f32(16)
        self.ld(cT.rearrange("p (r k) -> p r k", r=2), I["cc"].rearrange("r (k p) -> p r k", p=128), (), ["cT"],
                allow_slow_non_contiguous=True)
        cA = ar.f32(16)
        self.act(cA, cT, AF.Silu, ["cT"], ["cA"])
        self.cp("dve", self.cB.rearrange("p (k r) -> p r k", r=2), cA.rearrange("p (r k) -> p r k", r=2), ["cA"], ["cB"])
        S.barrier()
        ar.release(m0)
        gmark = ar.mark()
        for l in range(DEPTH):
            ar.release(gmark)
            self.phase_mods(l)
            S.barrier(); ar.release(gmark)
            if self.stop == f"mods{l}":
                break
            self.phase_p1(l)
            S.barrier(); ar.release(gmark)
            if self.stop == f"p1_{l}":
                break
            self.phase_tables(l)
            self.phase_p2(l)
            S.barrier()
            if self.stop == f"p2_{l}":
                break
            self.phase_p3(l)
            S.barrier(); ar.release(gmark)
            if self.stop == f"p3_{l}":
                break
            if l == 0:
                self.phase_ffn(l, "ctx", [0, 1], I["ffn_w_in"][0], I["ffn_w_out"][0], None)
                S.barrier(); ar.release(gmark)
                self.phase_ffn(l, "lat", list(range(2, 18)), I["ffn_w_in"][0], I["ffn_w_out"][0], None)
                S.barrier(); ar.release(gmark)
                self.phase_ffn(l, "lat", list(range(18, 34)), I["ffn_w_in"][0], I["ffn_w_out"][0], None)
                S.barrier(); ar.release(gmark)
                if self.stop == "l0":
                    break
            else:
                self.phase_ffn(l, "moe", list(range(16)), I["moe_w_in"][0], I["moe_w_out"][0], 0)
                S.barrier(); ar.release(gmark)
        if self.dbg:
            S.barrier()
            self.dbg[0](self)

    def hsrc(self, l, ti):
        if l == 0:
            if ti < 2:
                return self.I["ctx"][ti * 128:(ti + 1) * 128, :]
            return self.I["x"][(ti - 2) * 128:(ti - 1) * 128, :]
        return self.H[ti * 128:(ti + 1) * 128, :]

    def modrow(self, kind, j):
        r = 0 if kind == "lat" else 1
        return pbcast(self.MOD[r:r + 1, j * D:(j + 1) * D], D)

    def phase_mods(self, l):
        ar = self.ar
        I = self.I
        wb = [ar.bf16(8 * 512), ar.bf16(8 * 512)]
        bb = [ar.f32(512), ar.f32(512)]
        ob = [ar.f32(512), ar.f32(512)]
        cBv = self.cB.rearrange("p (k r) -> p k r", r=2)
        for n in range(12):
            s = n % 2
            self.ld(wb[s].rearrange("p (k n) -> p k n", k=8),
                    I["ada_w"][l][:, n * 512:(n + 1) * 512].rearrange("(k p) n -> p k n", p=128),
                    (), [f"mw{s}"], eng="pool")
            src = I["ada_b"][l:l + 1, n * 512:(n + 1) * 512]
            self.ld(bb[s][0:2, :], bass.AP(src.tensor, src.offset, [[0, 2], [1, 512]]), (), [f"mb{s}"])
            pb = self.pA[s]
            for k in range(8):
                self.mm(pb[0:2, :], cBv[:, k, :], wb[s][:, k * 512:(k + 1) * 512], k == 0, k == 7,
                        [f"mw{s}", "cB"], [self.pAn[s]])
            self.tt("dve", ob[s][0:2, :], pb[0:2, :], bb[s][0:2, :], ALU.add, [self.pAn[s], f"mb{s}"], [f"mo{s}"])
            self.ld(self.MOD[:, n * 512:(n + 1) * 512], ob[s][0:2, :], [f"mo{s}"], ["MOD"])

    def phase_p1(self, l):
        ar = self.ar
        I = self.I
        S = self.S
        w = ar.bf16(8 * DIN)
        wv = w.rearrange("p (k n) -> p k n", k=8)
        for k in range(8):
            self.ld(wv[:, k, :], I["w_in"][l][k * 128:(k + 1) * 128, :], (), ["w_in"], eng="pool",
                    max_dma_last_dim=4096)
        g1n = ar.f32(D)
        self.ld(g1n, pbcast(I["norm1_g"][l:l + 1, :], D), (), ["g1n"])
        A1 = {}; SH = {}
        for kind in ("lat", "ctx"):
            if l == 1 and kind == "ctx" and False:
                continue
            A1[kind] = ar.f32(D); SH[kind] = ar.f32(D)
            self.ld(SH[kind], self.modrow(kind, 0), ["MOD"], [f"sh_{kind}"])
            self.ld(A1[kind], self.modrow(kind, 1), ["MOD"], [f"a1_{kind}"])
            self.stt("dve", A1[kind], A1[kind], 1.0, g1n, ALU.add, ALU.mult, [f"a1_{kind}", "g1n"], [f"a1_{kind}"])
        z = ar.bf16(256)
        self.memset("pool", z[0:1, :], 0.0, ["zrow"])
        for r in (0, 257, NT * 128 + 2):
            self.ld(self.UU[r:r + 1, :], z[0:1, :], ["zrow"], ["UU"])
        hb = [ar.f32(D), ar.f32(D), ar.f32(D)]
        junk = ar.f32(D)
        stat = ar.f32(NT * 2)
        self.memset("pool", stat, 0.0, ["stat0", "stat1"])
        tmp = ar.f32(D)
        ab = [ar.bf16(D), ar.bf16(D)]
        aT = [ar.bf16(D), ar.bf16(D)]
        pr = ar.f32(DIN)
        rc = [ar.f32(64) for _ in range(3)]; rs = [ar.f32(64) for _ in range(3)]
        t1 = ar.f32(768); t2 = ar.f32(768)
        qb = [ar.bf16(384), ar.bf16(384)]
        tmo = [ar.bf16(1408), ar.bf16(1408)]
        ub = [ar.bf16(256), ar.bf16(256)]
        nb = [ar.bf16(1152), ar.bf16(1152)]
        qkT = [ar.bf16(768), ar.bf16(768)]
        nnT = [ar.bf16(768), ar.bf16(768)]
        pr2 = [pr, ar.f32(DIN)]
        bankctr = [0]

        def loads_a(ti):
            s3 = ti % 3
            self.ld(hb[s3], self.hsrc(l, ti), ["H"], [f"hb{s3}"])
            self.ld(rc[s3], I["rope_c"][ti * 128:(ti + 1) * 128, :], (), [f"rc{s3}"])
            self.ld(rs[s3], I["rope_s"][ti * 128:(ti + 1) * 128, :], (), [f"rs{s3}"])

        def stage_a(ti):
            s = ti % 2
            s3 = ti % 3
            kind = "ctx" if ti < 2 else "lat"
            ss = stat[:, 2 * ti:2 * ti + 1]; rstd = stat[:, 2 * ti + 1:2 * ti + 2]
            self.sumsq(junk, hb[s3], ss, [f"hb{s3}"], ["junk", f"stat{s}"])
            self.act(rstd, ss, AF.Sqrt, [f"stat{s}"], [f"stat{s}"], bias=EPS, scale=1.0 / D)
            self.S.add("dve", lambda e, rstd=rstd: e.reciprocal(rstd, rstd), [f"stat{s}"], [f"stat{s}"])
            self.stt("dve", tmp, hb[s3], rstd, A1[kind], ALU.mult, ALU.mult, [f"hb{s3}", f"stat{s}", f"a1_{kind}"], ["tmp"])
            self.tt("pool", ab[s], tmp, SH[kind], ALU.add, ["tmp", f"sh_{kind}"], [f"ab{s}"])

        def stage_a_tr(ti):
            s = ti % 2
            tb = 0
            for k in range(8):
                self.tr(self.pT[tb][:, k * 128:(k + 1) * 128], ab[s][:, k * 128:(k + 1) * 128], self.ident,
                        [f"ab{s}", "ident"], [self.pTn[tb]])
            self.cp("act", aT[s], self.pT[tb], [self.pTn[tb]], [f"aT{s}"])

        def stage_b(ti, hooks):
            s = ti % 2
            p_ = pr2[s]
            for n in range(7):
                if n in hooks:
                    hooks[n]()
                cols = 512 if n < 6 else 384
                bk = bankctr[0] % 6
                bankctr[0] += 1
                for k in range(8):
                    self.mm(self.pA[bk][:, 0:cols], aT[s][:, k * 128:(k + 1) * 128], wv[:, k, n * 512:n * 512 + cols],
                            k == 0, k == 7, [f"aT{s}", "w_in"], [self.pAn[bk]])
                self.cp("act" if n % 2 == 0 else "dve", p_[:, n * 512:n * 512 + cols], self.pA[bk][:, 0:cols],
                        [self.pAn[bk]], [f"pr{s}_{n}"])

        def stage_c1(ti):
            s = ti % 2
            s3 = ti % 3
            p_ = pr2[s]
            P = lambda *ns: [f"pr{s}_{n}" for n in ns]
            qk3 = p_[:, 0:768].rearrange("p (h d) -> p h d", d=64)
            self.tt("dve", t1.rearrange("p (h d) -> p h d", d=64), qk3,
                    rc[s3].unsqueeze(1).broadcast_to([128, 12, 64]), ALU.mult, P(0, 1) + [f"rc{s3}"], ["t1"])
            qk5 = p_[:, 0:768].rearrange("p (h c x d) -> p h c x d", c=2, x=2, d=16)
            t25 = t2.rearrange("p (h c x d) -> p h c x d", c=2, x=2, d=16)
            rs4 = rs[s3].rearrange("p (c x d) -> p c x d", c=2, x=2)
            for xx in range(2):
                self.tt("pool", t25[:, :, :, xx, :], qk5[:, :, :, 1 - xx, :],
                        rs4[:, :, xx, :].unsqueeze(1).broadcast_to([128, 12, 2, 16]), ALU.mult,
                        P(0, 1) + [f"rs{s3}"], ["t2"])
            self.tt("dve", qb[s], t1[:, 0:384], t2[:, 0:384], ALU.add, ["t1", "t2"], [f"qb{s}"])
            self.tt("dve", tmo[s][:, 0:384], t1[:, 384:768], t2[:, 384:768], ALU.add, ["t1", "t2"], [f"tmo{s}"])
            self.cp("pool", tmo[s][:, 384:768], p_[:, 768:1152], P(1, 2), [f"tmo{s}"])
            self.act(tmo[s][:, 768:1152], p_[:, 1152:1536], AF.Silu, P(2), [f"tmo{s}"])
            self.cp("pool", tmo[s][:, 1152:1408], p_[:, 1536:1792], P(3), [f"tmo{s}"])
            self.tt("pool", ub[s], p_[:, 1792:2048], p_[:, 2048:2304], ALU.mult, P(3, 4), [f"ub{s}"])
            self.cp("dve", nb[s], p_[:, 2304:3456], P(4, 5, 6), [f"nb{s}"])
            self.ld(self.TM[ti * 128:(ti + 1) * 128, :], tmo[s], [f"tmo{s}"], ["TM"], eng=_P1ST)
            urow = 1 + ti * 128 if ti < 2 else 2 + ti * 128
            self.ld(self.UU[urow:urow + 128, :], ub[s], [f"ub{s}"], ["UU"], eng=_P1ST)
            self.ld(self.NV[ti * 128:(ti + 1) * 128, :], nb[s][:, 768:1152], [f"nb{s}"], ["NV"], eng=_P1ST)

        def stage_c2(ti):
            s = ti % 2
            tb = 1
            for g in range(3):
                self.tr(self.pT[tb][:, g * 128:(g + 1) * 128], qb[s][:, g * 128:(g + 1) * 128], self.ident,
                        [f"qb{s}", "ident"], [self.pTn[tb]])
                self.tr(self.pT[tb][:, 384 + g * 128:384 + (g + 1) * 128], tmo[s][:, g * 128:(g + 1) * 128], self.ident,
                        [f"tmo{s}", "ident"], [self.pTn[tb]])
            self.cp("act", qkT[s], self.pT[tb][:, 0:768], [self.pTn[tb]], [f"qkT{s}"])
            self.ld(self.QT[ti], qkT[s][:, 0:384], [f"qkT{s}"], ["QT"], eng=_P1ST)
            self.ld(self.KT[ti], qkT[s][:, 384:768], [f"qkT{s}"], ["KT"], eng=_P1ST)
            for g in range(3):
                self.tr(self.pT[tb][:, g * 128:(g + 1) * 128], nb[s][:, g * 128:(g + 1) * 128], self.jrev,
                        [f"nb{s}", "jrev"], [self.pTn[tb]])
                self.tr(self.pT[tb][:, 384 + g * 128:384 + (g + 1) * 128], nb[s][:, 384 + g * 128:384 + (g + 1) * 128],
                        self.ident, [f"nb{s}", "ident"], [self.pTn[tb]])
            self.cp("dve", nnT[s], self.pT[tb][:, 0:768], [self.pTn[tb]], [f"nnT{s}"])
            self.ld(self.NQT[ti], nnT[s][:, 0:384], [f"nnT{s}"], ["NQT"], eng=_P1ST)
            self.ld(self.NKT[:, :, ti * 128:(ti + 1) * 128].rearrange("g p t -> p g t"),
                    nnT[s][:, 384:768].rearrange("p (g t) -> p g t", g=3), [f"nnT{s}"], ["NKT"], eng=_P1ST)

        loads_a(0)
        loads_a(1)
        stage_a(0)
        stage_a_tr(0)
        for ti in range(NT):
            hooks = {}
            if ti + 2 < NT:
                loads_a(ti + 2)
            if ti + 1 < NT:
                stage_a(ti + 1)
                hooks[3] = (lambda t=ti + 1: stage_a_tr(t))
            if ti >= 1:
                hooks[5] = (lambda t=ti - 1: stage_c2(t))
            stage_b(ti, hooks)
            stage_c1(ti)
        stage_c2(NT - 1)

    def phase_tables(self, l):
        ar = self.ar
        I = self.I
        T = {}
        self.T = T
        lgc = ar.f32(12)
        src = I["rdl"][l]
        self.ld(lgc, bass.AP(src.tensor, src.offset, [[0, 128], [1, 12]]), (), ["lgc"])
        lgp = ar.f32(6)
        for half in range(2):
            self.ld(lgp[half * 64:(half + 1) * 64, :].rearrange("p (d g) -> p d g", d=2),
                    bass.AP(src.tensor, src.offset + half, [[0, 64], [6, 2], [2, 3]]), (), ["lgp"],
                    allow_slow_non_contiguous=True)
        for nm, t, n in (("lgc", lgc, 12), ("lgp", lgp, 6)):
            self.act(t, t, AF.Exp, [nm], [nm], scale=-1.0)
            self.act(t, t, AF.Ln, [nm], [nm], bias=1.0, scale=1.0)
            self.ts("dve", t, t, -1.0, None, ALU.mult, None, [nm], [nm])
        pos4 = ar.f32(4)
        self.ld(pos4, I["pos4"], (), ["pos4"])
        io = ar.f32(256)
        self.ld(io, I["iota12"].rearrange("p a b -> p (a b)"), (), ["io"])
        dt = ar.f32(512)
        self.ld(dt, I["dtab"].rearrange("p a b -> p (a b)"), (), ["dt"])
        T["bm"] = ar.f32(128)
        self.ld(T["bm"], I["blockmask"], (), ["bm"])
        e6 = ar.f32(12)
        self.act(e6[:, 0:6], lgc[:, 0:6], AF.Exp, ["lgc", "pos4"], ["e6"], scale=pos4[:, 1:2])
        self.act(e6[:, 6:12], lgc[:, 6:12], AF.Exp, ["lgc", "pos4"], ["e6"], scale=pos4[:, 0:1])
        T["kd"] = ar.f32(768)
        self.ts("dve", T["kd"].rearrange("p (h d) -> p h d", d=64), e6.unsqueeze(2).broadcast_to([128, 12, 64]),
                0.125, None, ALU.mult, None, ["e6"], ["kd"])
        T["dq"] = ar.f32(768)
        for d_ in range(2):
            for g in range(3):
                o = (d_ * 3 + g) * 128
                self.act(T["dq"][:, o:o + 128], io[:, d_ * 128:(d_ + 1) * 128], AF.Exp, ["lgp", "io"], ["dq"],
                         scale=lgp[:, d_ * 3 + g:d_ * 3 + g + 1])
        T["gam"] = ar.f32(6)
        self.act(T["gam"], lgp, AF.Exp, ["lgp"], ["gam"], scale=128.0)
        T["M"] = ar.f32(768)
        ef = ar.f32(128); eb = ar.f32(128)
        for h in range(6):
            self.act(ef, dt[:, 0:128], AF.Exp, ["lgc", "dt"], ["ef"], scale=lgc[:, h:h + 1])
            self.act(eb, dt[:, 128:256], AF.Exp, ["lgc", "dt"], ["eb"], scale=lgc[:, 6 + h:7 + h])
            self.tt("dve", ef, ef, dt[:, 256:384], ALU.mult, ["ef", "dt"], ["ef"])
            self.tt("dve", eb, eb, dt[:, 384:512], ALU.mult, ["eb", "dt"], ["eb"])
            self.tt("dve", ef, ef, eb, ALU.add, ["ef", "eb"], ["ef"])
            self.ts("dve", T["M"][:, h * 128:(h + 1) * 128], ef, 0.125, None, ALU.mult, None, ["ef"], ["M"])
        T["gng"] = ar.f32(384)
        self.ld(T["gng"], pbcast(I["gng"][l:l + 1, :], 384), (), ["gng"])
        T["cw"] = ar.f32(768)
        srcw = I["conv_w"][l]
        self.ld(T["cw"], bass.AP(srcw.tensor, srcw.offset, [[0, 128], [1, 768]]), (), ["cw"])
        m0 = ar.mark()
        T["bint"] = None
        pz = ar.f32(127)
        self.memset("pool", pz[0:90, :], 0.0, ["pz"])
        self.ld(pz[0:90, 48:79], I["rpb"][l].rearrange("h r c -> (h r) c"), (), ["pz"])
        self.ld(self.PZ, pz[0:90, :], ["pz"], ["PZ"])
        T["bint"] = ar.f32(6 * 576)
        m1 = ar.mark()
        tz = ar.f32(90 * 64)
        for half in range(2):
            self.ld(tz[half * 64:(half + 1) * 64, :].rearrange("p (r k) -> p r k", k=64),
                    bass.AP(self.PZ.tensor, 0, [[1, 64], [127, 90], [1, 64]]), ["PZ"], ["tz"])
        cm = ar.f32(64)
        self.ld(cm, I["na_cmask"], (), ["cm"])
        tz3 = tz.rearrange("p (r k) -> p r k", k=64)
        be = ar.f32(6 * 576)
        pats = [((3, 2), ((0, 8), (1, 9)), 9), ((7, 6), ((0, 8), (0, 8)), 8), ((5, 4), ((0, 8), (0, 8)), 8),
                ((3, 2), ((0, 8), (0, 8)), 8), ((1, 0), ((0, 8), (0, 8)), 8)]
        for pi, (offs, val, nrows) in enumerate(pats):
            dst = T["bint"] if pi == 0 else be
            dname = "bint" if pi == 0 else "be"
            d4 = dst.rearrange("p (h r k) -> p h r k", h=6, k=64)
            if pi == 0:
                self.memset("pool", dst, NEG, [dname])
            for h in range(6):
                for a in range(2):
                    lo, hi = val[a]
                    ps = slice(a * 64, (a + 1) * 64)
                    r0 = h * 15 + offs[a] + lo
                    self.tt("dve", d4[ps, h, lo:hi, :], tz3[ps, r0:r0 + (hi - lo), :],
                            cm[ps, :].unsqueeze(1).broadcast_to([64, hi - lo, 64]), ALU.add, ["tz", "cm"], [dname])
            if pi > 0:
                self.ld(self.BE[pi - 1], be, ["be"], ["BE"])
        self.S.barrier()
        ar.release(m1)

    def phase_p2(self, l):
        ar = self.ar
        T = self.T
        m0 = ar.mark()
        kvall = ar.bf16(NT * 768)
        for ti in range(NT):
            self.ld(kvall[:, ti * 768:(ti + 1) * 768], self.TM[ti * 128:(ti + 1) * 128, 0:768], ["TM"], [f"kv{ti}"])
        kd = [ar.bf16(384) for _ in range(2)]
        um = [ar.f32(384) for _ in range(2)]
        Sst = [ar.f32(384) for _ in range(2)]
        sb = [ar.bf16(384) for _ in range(4)]
        for d_ in range(2):
            self.memset("pool", Sst[d_], 0.0, [f"S{d_}"])
        order = [list(range(NT)), [1, 0] + list(range(NT - 1, 1, -1))]
        bmb = T["bm"].unsqueeze(1).broadcast_to([128, 3, 128])
        for step in range(NT):
            for d_ in range(2):
                ti = order[d_][step]
                s = (step * 2 + d_) % 4
                kvt = kvall[:, ti * 768:(ti + 1) * 768]
                self.cp("act", sb[s], Sst[d_], [f"S{d_}"], [f"sb{s}"])
                self.ld(self.ST[ti, d_], sb[s], [f"sb{s}"], ["ST"])
                self.tt("pool", kd[d_], kvt[:, 0:384], T["kd"][:, d_ * 384:(d_ + 1) * 384], ALU.mult,
                        [f"kv{ti}", "kd"], [f"kdb{d_}"])
                pb = self.pA[d_]
                for g in range(3):
                    self.mm(pb[:, g * 128:(g + 1) * 128], kd[d_][:, g * 128:(g + 1) * 128],
                            kvt[:, 384 + g * 128:384 + (g + 1) * 128], True, True,
                            [f"kdb{d_}", f"kv{ti}"], [self.pAn[d_]])
                self.tt("dve", um[d_].rearrange("p (g n) -> p g n", g=3), pb[:, 0:384].rearrange("p (g n) -> p g n", g=3),
                        bmb, ALU.mult, [self.pAn[d_], "bm"], [f"um{d_}"])
                for g in range(3):
                    self.stt("dve", Sst[d_][:, g * 128:(g + 1) * 128], Sst[d_][:, g * 128:(g + 1) * 128],
                             T["gam"][:, d_ * 3 + g:d_ * 3 + g + 1], um[d_][:, g * 128:(g + 1) * 128],
                             ALU.mult, ALU.add, [f"S{d_}", "gam", f"um{d_}"], [f"S{d_}"])
        ar.release(m0)

    def phase_p3(self, l):
        ar = self.ar
        I = self.I
        T = self.T
        tiles = list(range(NT)) if l == 0 else list(range(2, NT))
        wo = ar.bf16(8 * D)
        wov = wo.rearrange("p (k n) -> p k n", k=8)
        for k in range(8):
            self.ld(wov[:, k, :], I["w_out"][l][k * 128:(k + 1) * 128, :], (), ["w_out"], eng="pool")
        G1 = {}
        for kind in (("lat", "ctx") if l == 0 else ("lat",)):
            G1[kind] = ar.f32(D)
            self.ld(G1[kind], self.modrow(kind, 2), ["MOD"], [f"g1_{kind}"])
        nkc = ar.bf16(3 * 256)
        self.ld(nkc.rearrange("p (g t) -> p g t", g=3), self.NKT[:, :, 0:256].rearrange("g p t -> p g t"), ["NKT"], ["nkc"])
        nvc = ar.bf16(2 * 384)
        self.ld(nvc.rearrange("p (j c) -> p j c", j=2), self.NV[0:256, :].rearrange("(j p) c -> p j c", p=128), ["NV"], ["nvc"])
        bedge = ar.f32(6 * 576)
        jrf = ar.f32(128)
        self.ld(jrf, I["jrev_f"], (), ["jrf"])
        nbuf = 2
        qt = [ar.bf16(384) for _ in range(nbuf)]; kt = [ar.bf16(384) for _ in range(nbuf)]
        stf = [ar.bf16(384) for _ in range(nbuf)]; stb = [ar.bf16(384) for _ in range(nbuf)]
        vgc = [ar.bf16(1024) for _ in range(nbuf)]
        u3 = [ar.bf16(768) for _ in range(nbuf)]
        nqt = [ar.bf16(384) for _ in range(nbuf)]
        nkw = [ar.bf16(3 * 576) for _ in range(nbuf)]
        nvw = [ar.bf16(5 * 384) for _ in range(nbuf)]
        hb = [ar.f32(D) for _ in range(nbuf)]
        qf = ar.bf16(384); qbk = ar.bf16(384)
        A = ar.bf16(768)
        osb = ar.f32(384); osq = ar.f32(384)
        stt_ = ar.f32(48)
        ycat = [ar.bf16(D) for _ in range(2)]
        c1 = ar.f32(256); c2 = ar.f32(256)
        sc = [ar.f32(832) for _ in range(2)]
        pb_ = [ar.bf16(832) for _ in range(2)]
        pts = [ar.bf16(896) for _ in range(2)]
        nst = ar.f32(NT * 6 * 4)
        self.memset("pool", nst, 0.0, ["nst3"] + [f"nm{h}" for h in range(6)] + [f"nr{h}" for h in range(6)])
        yT = ar.bf16(D)
        h1 = [ar.f32(D) for _ in range(2)]
        wtmp = ar.f32(D)
        scale = 0.125

        def loads(ti):
            s = ti % nbuf
            R = {}
            self.ld(qt[s], self.QT[ti], ["QT"], [f"qt{s}"])
            self.ld(kt[s], self.KT[ti], ["KT"], [f"kt{s}"])
            self.ld(stf[s], self.ST[ti, 0], ["ST"], [f"stf{s}"])
            self.ld(stb[s], self.ST[ti, 1], ["ST"], [f"stb{s}"])
            self.ld(vgc[s], self.TM[ti * 128:(ti + 1) * 128, 384:1408], ["TM"], [f"vgc{s}"])
            urow = 1 + ti * 128 if ti < 2 else 2 + ti * 128
            self.ld(u3[s].rearrange("p (j c) -> p j c", j=3),
                    bass.AP(self.UU.tensor, (urow - 1) * 256, [[256, 128], [256, 3], [1, 256]]), ["UU"], [f"u3{s}"])
            self.ld(nqt[s], self.NQT[ti], ["NQT"], [f"nqt{s}"])
            self.ld(hb[s], self.hsrc(l, ti), ["H"], [f"h3{s}"])
            if ti >= 2:
                c = ti - 2
                if c <= 1:
                    R0, nrows, pat = 0, 8, 1 + c
                elif c >= 30:
                    R0, nrows, pat = 56, 8, 3 + (c - 30)
                else:
                    R0, nrows, pat = 2 * c - 4, 9, 0
                t0 = 256 + R0 * 64
                nk = nrows * 64
                self.ld(nkw[s].rearrange("p (g t) -> p g t", g=3)[:, :, 0:nk],
                        self.NKT[:, :, t0:t0 + nk].rearrange("g p t -> p g t"), ["NKT"], [f"nkw{s}"])
                self.ld(nvw[s].rearrange("p (j c) -> p j c", j=5)[:, 0:4, :],
                        self.NV[t0:t0 + 512, :].rearrange("(j p) c -> p j c", p=128), ["NV"], [f"nvw{s}"])
                if nrows == 9:
                    self.ld(nvw[s][0:64, 4 * 384:5 * 384], self.NV[t0 + 512:t0 + 576, :], ["NV"], [f"nvw{s}"])
                return (nrows, pat)
            return (0, -1)

        X, Y, Z = 0, 1, 2

        def ret_part(ti):
            s = ti % nbuf
            dq = T["dq"]
            self.tt("pool", qf, qt[s], dq[:, 0:384], ALU.mult, [f"qt{s}", "dq"], ["qf"])
            self.tt("pool", qbk, qt[s], dq[:, 384:768], ALU.mult, [f"qt{s}", "dq"], ["qbk"])
            X, Y, Z = 0, 1, 2
            for h in range(6):
                g, hf = h // 2, h % 2
                psl = slice(hf * 64, (hf + 1) * 64)
                bank, col = (X, h * 128) if h < 4 else (Y, (h - 4) * 128)
                self.mm(self.pA[bank][:, col:col + 128], kt[s][psl, g * 128:(g + 1) * 128],
                        qt[s][psl, g * 128:(g + 1) * 128], True, True, [f"kt{s}", f"qt{s}"], [self.pAn[bank]], serial=True)
            self.tt("dve", A[:, 0:512], self.pA[X][:, 0:512], T["M"][:, 0:512], ALU.mult, [self.pAn[X], "M"], ["A"])
            self.tt("dve", A[:, 512:768], self.pA[Y][:, 0:256], T["M"][:, 512:768], ALU.mult, [self.pAn[Y], "M"], ["A"])
            for g in range(3):
                cs = slice(g * 128, (g + 1) * 128)
                self.mm(self.pA[Z][:, cs], qf[:, cs], stf[s][:, cs], True, False, ["qf", f"stf{s}"], [self.pAn[Z]])
                self.mm(self.pA[Z][:, cs], qbk[:, cs], stb[s][:, cs], False, False, ["qbk", f"stb{s}"], [self.pAn[Z]])
                for h in (2 * g, 2 * g + 1):
                    self.mm(self.pA[Z][:, h * 64:(h + 1) * 64], A[:, h * 128:(h + 1) * 128], vgc[s][:, h * 64:(h + 1) * 64],
                            False, h == 2 * g + 1, ["A", f"vgc{s}"], [self.pAn[Z]])

        info = {}
        info[tiles[0]] = loads(tiles[0])
        _hoist = _os.environ.get("P3_HOIST", "0") == "1"
        _hoist2 = _os.environ.get("P3_HOIST", "2") == "2"
        if _hoist or _hoist2:
            ret_part(tiles[0])
        for ix, ti in enumerate(tiles):
            s = ti % nbuf
            ys = ix % 2
            kind = "ctx" if ti < 2 else "lat"
            if ix + 1 < len(tiles):
                info[tiles[ix + 1]] = loads(tiles[ix + 1])
            nrows, pat = info[ti]
            if pat > 0:
                self.ld(bedge, self.BE[pat - 1], ["BE"], ["bedge"])
            if not (_hoist or _hoist2):
                ret_part(ti)
            nk = nrows * 64
            ln = 256 + nk
            nch = (ln + 127) // 128
            bias = T["bint"] if pat == 0 else bedge
            bname = "bint" if pat == 0 else "bedge"
            PA, PB, PC = 3, 4, 5
            o = ti * 24
            nkc3 = nkc.rearrange("p (g t) -> p g t", g=3)
            nkw3 = nkw[s].rearrange("p (g t) -> p g t", g=3)
            nvc3 = nvc.rearrange("p (j c) -> p j c", j=2)
            nvw3 = nvw[s].rearrange("p (j c) -> p j c", j=5)

            def na_s1(h):
                g, hf = h // 2, h % 2
                psl = slice(hf * 64, (hf + 1) * 64)
                hs = h % 2
                q = nqt[s][psl, g * 128:(g + 1) * 128]
                self.mm(self.pA[PB][:, 0:256], q, nkc3[psl, g, :], True, True, [f"nqt{s}", "nkc"], [self.pAn[PB]], serial=True)
                if nk:
                    self.mm(self.pA[PB][:, 256:512], q, nkw3[psl, g, 0:256], True, True, [f"nqt{s}", f"nkw{s}"], [self.pAn[PB]],
                            serial=True)
                    self.mm(self.pA[PA][:, 0:nk - 256], q, nkw3[psl, g, 256:nk], True, True, [f"nqt{s}", f"nkw{s}"], [self.pAn[PA]],
                            serial=True)
                self.ts("dve", sc[hs][:, 0:256], self.pA[PB][:, 0:256], scale, None, ALU.mult, None,
                        [self.pAn[PB]], [f"sc{hs}"])
                if nk:
                    self.stt("dve", sc[hs][:, 256:512], self.pA[PB][:, 256:512], scale, bias[:, h * 576:h * 576 + 256],
                             ALU.mult, ALU.add, [self.pAn[PB], bname], [f"sc{hs}"])
                    self.stt("dve", sc[hs][:, 512:ln], self.pA[PA][:, 0:nk - 256], scale, bias[:, h * 576 + 256:h * 576 + nk],
                             ALU.mult, ALU.add, [self.pAn[PA], bname], [f"sc{hs}"])
                mx = nst[:, o + h:o + h + 1]; nmx = nst[:, o + 6 + h:o + 7 + h]
                self.red(mx, sc[hs][:, 0:ln], ALU.max, [f"sc{hs}"], [f"nm{h}"])
                self.ts("dve", nmx, mx, -1.0, None, ALU.mult, None, [f"nm{h}"], [f"nm{h}"])

            def na_s2(h):
                hs = h % 2
                nmx = nst[:, o + 6 + h:o + 7 + h]; rsum = nst[:, o + 12 + h:o + 13 + h]
                self.act(pb_[hs][:, 0:ln], sc[hs][:, 0:ln], AF.Exp, [f"sc{hs}", f"nm{h}"], [f"pb{hs}", f"nr{h}"], bias=nmx, scale=1.0,
                         accum_out=rsum)
                tb = hs
                for ch in range(nch):
                    w_ = min(128, ln - ch * 128)
                    self.tr(self.pT[tb][0:w_, ch * 128:(ch + 1) * 128], pb_[hs][:, ch * 128:ch * 128 + w_], self.jrev,
                            [f"pb{hs}", "jrev"], [self.pTn[tb]])
                nfull = ln // 128
                self.cp("act", pts[hs][:, 0:nfull * 128], self.pT[tb][:, 0:nfull * 128], [self.pTn[tb]], [f"pts{hs}"])
                if nch > nfull:
                    self.cp("act", pts[hs][0:64, nfull * 128:nch * 128], self.pT[tb][0:64, nfull * 128:nch * 128],
                            [self.pTn[tb]], [f"pts{hs}"])

            def na_s3(h):
                hs = h % 2
                oc = slice(h * 64, (h + 1) * 64)
                for ch in range(nch):
                    w_ = min(128, ln - ch * 128)
                    rhs = nvc3[0:w_, ch, oc] if ch < 2 else nvw3[0:w_, ch - 2, oc]
                    self.mm(self.pA[PC][:, oc], pts[hs][0:w_, ch * 128:(ch + 1) * 128], rhs, ch == 0, ch == nch - 1,
                            [f"pts{hs}", "nvc", f"nvw{s}"], [self.pAn[PC]], serial=(w_ < 128))

            na_s1(0)
            self.cp("act", osb, self.pA[Z][:, 0:384], [self.pAn[Z]], ["osb"])
            self.act(osq, osb, AF.Square, ["osb"], ["osq"])
            o3 = osb.rearrange("p (h d) -> p h d", d=64)
            self.red(stt_[:, 0:6], o3, ALU.add, ["osb"], ["st_a"])
            self.red(stt_[:, 6:12], osq.rearrange("p (h d) -> p h d", d=64), ALU.add, ["osq"], ["st_b"])
            self.ts("dve", stt_[:, 12:18], stt_[:, 0:6], 1.0 / 64, None, ALU.mult, None, ["st_a"], ["st_c"])
            self.tt("dve", stt_[:, 18:24], stt_[:, 12:18], stt_[:, 12:18], ALU.mult, ["st_c"], ["st_d"])
            self.stt("dve", stt_[:, 24:30], stt_[:, 6:12], 1.0 / 64, stt_[:, 18:24], ALU.mult, ALU.subtract,
                     ["st_b", "st_d"], ["st_e"])
            self.act(stt_[:, 30:36], stt_[:, 24:30], AF.Sqrt, ["st_e"], ["st_f"], bias=EPS, scale=1.0)
            self.S.add("dve", lambda e: e.reciprocal(stt_[:, 36:42], stt_[:, 30:36]), ["st_f"], ["st_g"])
            self.tt("dve", o3, o3, stt_[:, 12:18].unsqueeze(2).broadcast_to([128, 6, 64]), ALU.subtract,
                    ["osb", "st_c"], ["osb"])
            self.tt("dve", o3, o3, stt_[:, 36:42].unsqueeze(2).broadcast_to([128, 6, 64]), ALU.mult,
                    ["osb", "st_g"], ["osb"])
            self.tt("pool", osb, osb, T["gng"], ALU.mult, ["osb", "gng"], ["osb"])
            self.tt("pool", ycat[ys][:, 0:384], osb, vgc[s][:, 384:768], ALU.mult, ["osb", f"vgc{s}"], [f"ycat{ys}"])
            cw = T["cw"]
            self.tt("pool", c1, u3[s][:, 0:256], cw[:, 0:256], ALU.mult, [f"u3{s}", "cw"], ["c1"])
            self.tt("pool", c2, u3[s][:, 256:512], cw[:, 256:512], ALU.mult, [f"u3{s}", "cw"], ["c2"])
            self.tt("pool", c1, c1, c2, ALU.add, ["c1", "c2"], ["c1"])
            self.tt("pool", c2, u3[s][:, 512:768], cw[:, 512:768], ALU.mult, [f"u3{s}", "cw"], ["c2"])
            self.tt("pool", c1, c1, c2, ALU.add, ["c1", "c2"], ["c1"])
            self.tt("pool", ycat[ys][:, 384:640], c1, vgc[s][:, 768:1024], ALU.mult, ["c1", f"vgc{s}"], [f"ycat{ys}"])
            na_s1(1)
            na_s2(0)
            for h in range(6):
                if h + 2 < 6:
                    na_s1(h + 2)
                if h + 1 < 6:
                    na_s2(h + 1)
                na_s3(h)
            if _hoist2 and ix + 1 < len(tiles):
                ret_part(tiles[ix + 1])
            self.mm(self.pA[PC][:, 384:390], jrf, nst[:, o + 12:o + 18], True, True, ["jrf"] + [f"nr{h}" for h in range(6)],
                    [self.pAn[PC]])
            self.S.add("dve", lambda e, o=o: e.reciprocal(nst[:, o + 18:o + 24], self.pA[PC][:, 384:390]), [self.pAn[PC]], ["nst3"])
            self.tt("dve", ycat[ys][:, 640:1024].rearrange("p (h d) -> p h d", d=64),
                    self.pA[PC][:, 0:384].rearrange("p (h d) -> p h d", d=64),
                    nst[:, o + 18:o + 24].unsqueeze(2).broadcast_to([128, 6, 64]), ALU.mult,
                    [self.pAn[PC], "nst3"], [f"ycat{ys}"])
            if self.dbg:
                self.ld(self.YC[ti * 128:(ti + 1) * 128, :], ycat[ys], [f"ycat{ys}"], ["YC"])
            if _hoist and ix + 1 < len(tiles):
                ret_part(tiles[ix + 1])
            tb = 0
            for k in range(8):
                self.tr(self.pT[tb][:, k * 128:(k + 1) * 128], ycat[ys][:, k * 128:(k + 1) * 128], self.ident,
                        [f"ycat{ys}", "ident"], [self.pTn[tb]])
            self.cp("act", yT, self.pT[tb], [self.pTn[tb]], ["yT"])
            for n in range(2):
                bk = 3 + n
                for k in range(8):
                    self.mm(self.pA[bk], yT[:, k * 128:(k + 1) * 128], wov[:, k, n * 512:(n + 1) * 512], k == 0, k == 7,
                            ["yT", "w_out"], [self.pAn[bk]])
                self.tt("dve", wtmp[:, n * 512:(n + 1) * 512], self.pA[bk], G1[kind][:, n * 512:(n + 1) * 512], ALU.mult,
                        [self.pAn[bk], f"g1_{kind}"], ["wtmp"])
            self.tt("pool", h1[ys], wtmp, hb[s], ALU.add, ["wtmp", f"h3{s}"], [f"h1_{ys}"])
            self.ld(self.H1[ti * 128:(ti + 1) * 128, :], h1[ys], [f"h1_{ys}"], ["H1"])

    def phase_ffn(self, l, kind, tiles, w1, w2, moe):
        ar = self.ar
        I = self.I
        S = self.S
        nt = len(tiles)
        ntok = nt * 128
        mk = "ctx" if kind == "ctx" else "lat"
        last = kind == "moe"
        SH2 = ar.f32(D); A2 = ar.f32(D); G2 = ar.f32(D)
        a2T = ar.bf16(8 * ntok)
        a2T_off = ar.off - 4 * ntok
        a2Tv = a2T.rearrange("p (k t) -> p k t", k=8)
        yacc = ar.f32(nt * D)
        st = ar.f32(nt * 4)
        self.memset("pool", st, 0.0, ["st0", "st1"])
        self.memset("pool", yacc, 0.0, ["yacc"])
        idf = ar.f32(128)
        self.ld(idf, I["ident_f"], (), ["idf"])
        if last:
            sel = ar.f32(2)
            rwt = ar.f32(64)
            rbt = ar.f32(8)
            self.ld(sel, I["sel"], (), ["sel"])
            self.ld(rwt.rearrange("p (k e) -> p k e", k=8), I["rw"][0].rearrange("(k p) e -> p k e", p=128), (), ["rwt"])
            self.ld(rbt, pbcast(I["rb"][0:1, :], 8), (), ["rbt"])
            gates = ar.f32(nt * 8)
            rt = ar.f32(64)
        NFS = 4
        supers = [(f0, min(NFS, 22 - f0)) for f0 in range(0, 22, NFS)]
        hid_off = ar.off
        hid = [ar.bf16(NFS * 512) for _ in range(2)]
        sgb = [ar.bf16(512) for _ in range(2)]
        w1b = [None, None]; w2b = [None, None]
        w1b[0] = ar.bf16(8 * 2 * NFS * 128); w2b[0] = ar.bf16(NFS * D)
        alias_off = ar.off
        w1b[1] = ar.bf16(8 * 2 * NFS * 128); w2b[1] = ar.bf16(NFS * D)
        base = ar.ap

        def f32at(off, n=D):
            return base[:, off:off + n]

        ha = [f32at(alias_off), f32at(alias_off + D)]
        hb2 = [f32at(alias_off + 2 * D), f32at(alias_off + 3 * D)]
        a32 = [f32at(alias_off + 4 * D), f32at(alias_off + 5 * D)]
        a32T = f32at(hid_off)
        assert alias_off + 6 * D <= ar.off and hid_off + D <= alias_off

        self.ld(hb2[0], pbcast(I["norm2_g"][l:l + 1, :], D), (), ["hb20"])
        self.ld(SH2, self.modrow(mk, 3), ["MOD"], ["sh2"])
        self.ld(A2, self.modrow(mk, 4), ["MOD"], ["a2m"])
        self.ld(G2, self.modrow(mk, 5), ["MOD"], ["g2m"])
        self.stt("dve", A2, A2, 1.0, hb2[0], ALU.add, ALU.mult, ["a2m", "hb20"], ["a2m"])

        experts = list(range(NEXP)) if moe is not None else [None]
        wlist = [(e, f0, nf) for e in experts for (f0, nf) in supers]

        def load_w(i):
            e, f0, nf = wlist[i]
            ws = i % 2
            W1 = w1 if e is None else w1[e]
            W2 = w2 if e is None else w2[e]
            w1v = w1b[ws].rearrange("p (k u c) -> p k u c", k=8, u=2)
            for u in range(2):
                c0 = u * DFF + f0 * 128
                self.ld(w1v[:, :, u, 0:nf * 128], W1[:, c0:c0 + nf * 128].rearrange("(k p) c -> p k c", p=128),
                        (), [f"w1b{ws}"], eng="pool")
            w2v = w2b[ws].rearrange("p (f n) -> p f n", f=NFS)
            self.ld(w2v[:, 0:nf, :], W2[f0 * 128:(f0 + nf) * 128, :].rearrange("(f p) n -> p f n", p=128),
                    (), [f"w2b{ws}"], eng="pool")

        load_w(0)

        def load_h(ti, p, ha_, hb_):
            if not last:
                self.ld(ha_[p], self.H1[ti * 128:(ti + 1) * 128, :], ["H1"], [f"ha{p}"])
            else:
                r0 = (2 + ti) * 128
                r1 = (2 + 16 + ti) * 128
                self.ld(ha_[p], self.H1[r0:r0 + 128, :], ["H1"], [f"ha{p}"])
                self.ld(hb_[p], self.H1[r1:r1 + 128, :], ["H1"], [f"hb2{p}"])
                self.ts("dve", ha_[p], ha_[p], sel[:, 0:1], None, ALU.mult, None, [f"ha{p}", "sel"], [f"ha{p}"])
                self.stt("dve", ha_[p], hb_[p], sel[:, 1:2], ha_[p], ALU.mult, ALU.add, [f"hb2{p}", "sel", f"ha{p}"], [f"ha{p}"])

        load_h(tiles[0], 0, ha, hb2)
        for j, ti in enumerate(tiles):
            p = j % 2
            if j + 1 < nt:
                load_h(tiles[j + 1], (j + 1) % 2, ha, hb2)
            ss = st[:, 4 * j:4 * j + 1]; rstd = st[:, 4 * j + 1:4 * j + 2]
            self.sumsq(a32[p], ha[p], ss, [f"ha{p}"], [f"a32{p}", f"st{p}"])
            self.act(rstd, ss, AF.Sqrt, [f"st{p}"], [f"st{p}"], bias=EPS, scale=1.0 / D)
            self.S.add("dve", lambda e, rstd=rstd: e.reciprocal(rstd, rstd), [f"st{p}"], [f"st{p}"])
            self.stt("dve", a32[p], ha[p], rstd, A2, ALU.mult, ALU.mult, [f"ha{p}", f"st{p}", "a2m"], [f"a32{p}"])
            self.tt("pool", a32[p], a32[p], SH2, ALU.add, [f"a32{p}", "sh2"], [f"a32{p}"])
            for k in range(8):
                bk = 2 * p + k // 4
                self.tr(self.pA[bk][:, (k % 4) * 128:(k % 4 + 1) * 128], a32[p][:, k * 128:(k + 1) * 128], idf,
                        [f"a32{p}", "idf"], [self.pAn[bk]])
            for b2 in range(2):
                bk = 2 * p + b2
                if last:
                    self.cp("dve", a32T[:, b2 * 512:(b2 + 1) * 512], self.pA[bk], [self.pAn[bk]], ["a32T"])
                    self.cp("act", a2Tv[:, 4 * b2:4 * b2 + 4, j * 128:(j + 1) * 128],
                            a32T[:, b2 * 512:(b2 + 1) * 512].rearrange("p (k t) -> p k t", k=4), ["a32T"], ["a2T"])
                else:
                    self.cp("act", a2Tv[:, 4 * b2:4 * b2 + 4, j * 128:(j + 1) * 128],
                            self.pA[bk].rearrange("p (k t) -> p k t", k=4), [self.pAn[bk]], ["a2T"])
            if last:
                rw3 = rwt.rearrange("p (k e) -> p k e", k=8)
                lg = rt[:, 0:8]; eq1 = rt[:, 8:16]; lg2 = rt[:, 16:24]; eq2 = rt[:, 24:32]
                m1 = rt[:, 32:33]; m2 = rt[:, 33:34]; dd = rt[:, 34:35]; ee = rt[:, 35:36]; w1_ = rt[:, 36:37]; w2_ = rt[:, 37:38]
                for k in range(8):
                    self.mm(self.pA[4][:, 0:8], a32T[:, k * 128:(k + 1) * 128], rw3[:, k, :], k == 0, k == 7,
                            ["a32T", "rwt"], [self.pAn[4]])
                self.tt("dve", lg, self.pA[4][:, 0:8], rbt, ALU.add, [self.pAn[4], "rbt"], ["rt0"])
                self.red(m1, lg, ALU.max, ["rt0"], ["rt1"])
                self.ts("dve", eq1, lg, m1, None, ALU.is_equal, None, ["rt0", "rt1"], ["rt2"])
                self.stt("dve", lg2, eq1, NEG, lg, ALU.mult, ALU.add, ["rt2", "rt0"], ["rt3"])
                self.red(m2, lg2, ALU.max, ["rt3"], ["rt4"])
                self.ts("dve", eq2, lg2, m2, None, ALU.is_equal, None, ["rt3", "rt4"], ["rt5"])
                self.tt("dve", dd, m2, m1, ALU.subtract, ["rt4", "rt1"], ["rt6"])
                self.act(ee, dd, AF.Exp, ["rt6"], ["rt7"])
                self.ts("dve", w1_, ee, 1.0, None, ALU.add, None, ["rt7"], ["rt8"])
                self.S.add("dve", lambda e, w1_=w1_: e.reciprocal(w1_, w1_), ["rt8"], ["rt8"])
                self.tt("dve", w2_, ee, w1_, ALU.mult, ["rt7", "rt8"], ["rt9"])
                gj = gates[:, j * 8:(j + 1) * 8]
                self.ts("dve", gj, eq1, w1_, None, ALU.mult, None, ["rt2", "rt8"], ["gates"])
                self.stt("dve", gj, eq2, w2_, gj, ALU.mult, ALU.add, ["rt5", "rt9", "gates"], ["gates"])
        S.barrier()
        groups = [list(range(g0, min(g0 + 4, nt))) for g0 in range(0, nt, 4)]
        fcnt = 0
        ycnt = 0
        gcnt = 0
        for i, (e, f0, nf) in enumerate(wlist):
            ws = i % 2
            if i + 1 < len(wlist):
                load_w(i + 1)
            w1v = w1b[ws].rearrange("p (k u c) -> p k u c", k=8, u=2)
            w2v = w2b[ws].rearrange("p (f n) -> p f n", f=NFS)
            for grp in groups:
                hs = gcnt % 2
                gcnt += 1
                t0 = grp[0] * 128
                ng = len(grp) * 128
                hv = hid[hs].rearrange("p (f t) -> p f t", f=NFS)
                for fi in range(nf):
                    b0 = 2 * (fcnt % 2)
                    sgs = fcnt % 2
                    fcnt += 1
                    for u in range(2):
                        for k in range(8):
                            self.mm(self.pA[b0 + u][:, 0:ng], w1v[:, k, u, fi * 128:(fi + 1) * 128], a2Tv[:, k, t0:t0 + ng],
                                    k == 0, k == 7, [f"w1b{ws}", "a2T"], [self.pAn[b0 + u]])
                    self.act(sgb[sgs][:, 0:ng], self.pA[b0][:, 0:ng], AF.Silu, [self.pAn[b0]], [f"sgb{sgs}"])
                    self.tt("dve", hv[:, fi, 0:ng], sgb[sgs][:, 0:ng], self.pA[b0 + 1][:, 0:ng], ALU.mult,
                            [f"sgb{sgs}", self.pAn[b0 + 1]], [f"hid{hs}"])
                for jj, j in enumerate(grp):
                    for oh in range(2):
                        bk = 4 + ycnt % 2
                        ycnt += 1
                        for fi in range(nf):
                            self.mm(self.pA[bk], hv[:, fi, jj * 128:(jj + 1) * 128], w2v[:, fi, oh * 512:(oh + 1) * 512],
                                    fi == 0, fi == nf - 1, [f"hid{hs}", f"w2b{ws}"], [self.pAn[bk]])
                        ya = yacc[:, j * D + oh * 512:j * D + (oh + 1) * 512]
                        if e is None:
                            self.tt("dve", ya, self.pA[bk], ya, ALU.add, [self.pAn[bk], "yacc"], ["yacc"])
                        else:
                            self.stt("dve", ya, self.pA[bk], gates[:, j * 8 + e:j * 8 + e + 1], ya, ALU.mult, ALU.add,
                                     [self.pAn[bk], "gates", "yacc"], ["yacc"])
        S.barrier()
        cha = [f32at(a2T_off), f32at(a2T_off + D)]
        chb = [f32at(a2T_off + 2 * D), f32at(a2T_off + 3 * D)]
        cr = [f32at(a2T_off + 4 * D), f32at(a2T_off + 5 * D)]
        co = [f32at(a2T_off + 6 * D), f32at(a2T_off + 7 * D)]
        assert 8 * D <= 4 * ntok or nt < 16
        if nt < 16:
            cha = ha; chb = hb2; cr = a32
            co = [f32at(alias_off - 3 * D), f32at(alias_off - 2 * D)]
        if last:
            self.ld(SH2, pbcast(I["final_g"].rearrange("(o d) -> o d", o=1), D), (), ["sh2"])
        load_h(tiles[0], 0, cha, chb)
        for j, ti in enumerate(tiles):
            p = j % 2
            if j + 1 < nt:
                load_h(tiles[j + 1], (j + 1) % 2, cha, chb)
            self.tt("pool", cr[p], yacc[:, j * D:(j + 1) * D], G2, ALU.mult, ["yacc", "g2m"], [f"cr{p}"])
            self.tt("dve", cr[p], cr[p], cha[p], ALU.add, [f"cr{p}", f"ha{p}"], [f"cr{p}"])
            if not last:
                self.ld(self.H[ti * 128:(ti + 1) * 128, :], cr[p], [f"cr{p}"], ["H"])
            else:
                ss = st[:, 4 * j + 2:4 * j + 3]; rstd = st[:, 4 * j + 3:4 * j + 4]
                self.sumsq(co[p], cr[p], ss, [f"cr{p}"], [f"co{p}", f"st{p}"])
                self.act(rstd, ss, AF.Sqrt, [f"st{p}"], [f"st{p}"], bias=EPS, scale=1.0 / D)
                self.S.add("dve", lambda e, rstd=rstd: e.reciprocal(rstd, rstd), [f"st{p}"], [f"st{p}"])
                self.stt("dve", co[p], cr[p], rstd, SH2, ALU.mult, ALU.mult, [f"cr{p}", f"st{p}", "sh2"], [f"co{p}"])
                self.ld(self.out[j * 128:(j + 1) * 128, :], co[p], [f"co{p}"], ["OUT"])


def _consts():
    bf = ml_dtypes.bfloat16
    C = {}
    C["ident_bf"] = np.eye(128, dtype=np.float32).astype(bf)
    J = np.zeros((128, 128), np.float32)
    for n in range(128):
        J[(n // 64) * 64 + 63 - n % 64, n] = 1.0
    C["jrev_bf"] = J.astype(bf)
    C["jrev_f"] = J.copy()
    C["ident_f"] = np.eye(128, dtype=np.float32)
    t = np.arange(SEQ)
    row = (t // 64).astype(np.float32)
    col = (t % 64).astype(np.float32)
    inv = (np.float32(10000.0) ** (-np.arange(16, dtype=np.float32) / np.float32(16))).astype(np.float32)
    ar_ = (row[:, None] * inv).astype(np.float32)
    ac_ = (col[:, None] * inv).astype(np.float32)
    cr, sr, cc_, sc_ = np.cos(ar_), np.sin(ar_), np.cos(ac_), np.sin(ac_)
    rc = np.concatenate([cr, cr, cc_, cc_], 1).astype(np.float32)
    rs = np.concatenate([-sr, sr, -sc_, sc_], 1).astype(np.float32)
    C["rope_c"] = np.concatenate([np.ones((CTX, 64), np.float32), rc], 0)
    C["rope_s"] = np.concatenate([np.zeros((CTX, 64), np.float32), rs], 0)
    p = np.arange(128, dtype=np.float32)
    C["pos4"] = np.stack([p, 127 - p, p + 1, 128 - p], 1).astype(np.float32)
    i = np.arange(128, dtype=np.float32)
    C["iota12"] = np.broadcast_to(np.stack([i + 1, 128 - i], 0)[None], (128, 2, 128)).astype(np.float32).copy()
    jj = p[:, None]
    ii = i[None, :]
    C["dtab"] = np.stack([np.maximum(ii - jj, 0), np.maximum(jj - ii, 0), (ii >= jj).astype(np.float32),
                          (jj >= ii).astype(np.float32)], 1).astype(np.float32).copy()
    hp = (np.arange(128) < 64)
    C["blockmask"] = (hp[:, None] == hp[None, :]).astype(np.float32)
    cm = np.full((128, 64), NEG, np.float32)
    for pp in range(128):
        qc = 63 - (pp % 64)
        c0 = min(max(qc - 8, 0), 48)
        cm[pp, c0:c0 + 16] = 0.0
    C["na_cmask"] = cm
    return C


_CACHE = {}


def kernel(**inputs):
    x = np.ascontiguousarray(inputs["x"], dtype=np.float32)
    if "nc" not in _CACHE:
        _CACHE["nc"] = Builder().build()
        _CACHE["consts"] = _consts()
    nc = _CACHE["nc"]
    C = _CACHE["consts"]
    f = lambda k: np.ascontiguousarray(inputs[k], dtype=np.float32)
    shared = {
        "ada_w": f("ada_w"), "ada_b": f("ada_b"), "norm1_g": f("norm1_g"), "norm2_g": f("norm2_g"),
        "w_in": f("w_in"), "w_out": f("w_out"), "ret_decay_logit": f("ret_decay_logit"), "ret_gn_g": f("ret_gn_g"),
        "conv_w": f("conv_w"), "na_rpb": f("na_rpb"), "ffn_w_in": f("ffn_w_in"), "ffn_w_out": f("ffn_w_out"),
        "moe_router_w": f("moe_router_w"), "moe_router_b": f("moe_router_b"), "moe_w_in": f("moe_w_in"),
        "moe_w_out": f("moe_w_out"), "final_g": f("final_g"),
    }
    shared.update(C)
    c = f("c"); ctx = f("ctx"); c_ctx = f("c_ctx")
    in_maps = []
    for core in range(8):
        b, half = core // 2, core % 2
        m = dict(shared)
        m["x"] = x[b]
        m["ctx"] = ctx[b]
        m["cc"] = np.stack([c[b], c_ctx], 0)
        sel = np.zeros((128, 2), np.float32)
        sel[:, half] = 1.0
        m["sel"] = sel
        in_maps.append(m)
    res = run_bass_kernel_spmd(nc, in_maps, core_ids=list(range(8)))
    out = np.empty((4, SEQ, D), np.float32)
    for core in range(8):
        b, half = core // 2, core % 2
        out[b, half * 2048:(half + 1) * 2048] = res.results[core]["out"]
    return out
```

```python
import numpy as np
import ml_dtypes
import concourse.bass as bass
import concourse.mybir as mybir
from concourse.bass_utils import run_bass_kernel_spmd
from contextlib import ExitStack

F32 = mybir.dt.float32
BF16 = mybir.dt.bfloat16
ALU = mybir.AluOpType
AF = mybir.ActivationFunctionType
AX = mybir.AxisListType

D = 1024
SEQ = 4096
CTX = 256
NT = 34
DIN = 3456
DFF = 2816
NEXP = 8
NEG = -30000.0
EPS = 1e-6
DEPTH = 2

SEM_LIM = 12000
import os as _os
_PE_EXEMPT = _os.environ.get("TRK_PE_EXEMPT", "1") == "1"
_PRUNE = _os.environ.get("TRK_PRUNE", "1") == "1"
_P1ST = _os.environ.get("P1_STORE_ENG", "sp")
DMA_K = 8


class Buf:
    __slots__ = ("name", "w", "r")

    def __init__(self, name):
        self.name = name
        self.w = None
        self.r = []


class Op:
    __slots__ = ("eng", "kind", "fn", "deps", "idx", "eidx", "need_sig", "sig", "slot", "target", "didx", "force")

    def __init__(self, eng, kind, fn):
        self.eng = eng
        self.kind = kind
        self.fn = fn
        self.deps = []
        self.need_sig = False
        self.sig = None
        self.slot = None
        self.target = None
        self.didx = None
        self.force = False


class Sched:
    ENGS = ("sp", "act", "pool", "pe", "dve")

    def __init__(self, nc):
        self.nc = nc
        self.ops = []
        self.per_eng = {e: [] for e in self.ENGS}
        self.ndma = {e: 0 for e in self.ENGS}
        self.bufs = {}

    def buf(self, name):
        b = self.bufs.get(name)
        if b is None:
            b = Buf(name)
            self.bufs[name] = b
        return b

    def add(self, eng, fn, reads=(), writes=(), kind="cmp", serial=False):
        op = Op(eng, kind, fn)
        op.idx = len(self.ops)
        op.eidx = len(self.per_eng[eng])
        op.force = serial
        if kind == "dma":
            op.didx = self.ndma[eng]
            self.ndma[eng] += 1
            op.slot = op.didx % DMA_K
            op.target = 16 * (op.didx // DMA_K + 1)
        deps = set()
        rl = [self.buf(b) if isinstance(b, str) else b for b in reads]
        wl = [self.buf(b) if isinstance(b, str) else b for b in writes]
        for b in rl:
            for w_ in (b.w or ()):
                deps.add(w_)
        for b in wl:
            for w_ in (b.w or ()):
                if not (kind == "dma" and self.ops[w_].kind == "dma"):
                    deps.add(w_)
            for r in b.r:
                deps.add(r)
        for b in rl:
            if kind == "cmp" and _PRUNE:
                b.r = [r for r in b.r if not (self.ops[r].kind == "cmp" and self.ops[r].eng == eng)]
            b.r.append(op.idx)
        for b in wl:
            if kind == "dma" and b.w and all(self.ops[w_].kind == "dma" for w_ in b.w) and not b.r:
                b.w = b.w + [op.idx]
            else:
                b.w = [op.idx]
            b.r = []
        if serial and self.per_eng[eng]:
            prev = self.per_eng[eng][-1]
            if prev.kind != "bar":
                deps.add(prev.idx)
        deps.discard(op.idx)
        op.deps = sorted(deps)
        self.ops.append(op)
        self.per_eng[eng].append(op)
        return op

    def dma(self, eng, out, in_, reads=(), writes=(), **kw):
        return self.add(eng, lambda e: e.dma_start(out=out, in_=in_, **kw), reads, writes, kind="dma")

    def barrier(self):
        deps = []
        for e in self.ENGS:
            lst = self.per_eng[e]
            last_c = None
            slots = {}
            for op in reversed(lst):
                if op.kind == "cmp" and last_c is None:
                    last_c = op
                elif op.kind == "dma" and op.slot not in slots:
                    slots[op.slot] = op
                if last_c is not None and len(slots) == DMA_K:
                    break
            if last_c is not None:
                deps.append(last_c.idx)
            deps.extend(o.idx for o in slots.values())
        for e in self.ENGS:
            op = Op(e, "bar", None)
            op.idx = len(self.ops)
            op.eidx = len(self.per_eng[e])
            op.deps = sorted(deps)
            self.ops.append(op)
            self.per_eng[e].append(op)

    def emit(self):
        nc = self.nc
        ops = self.ops
        need = {}
        for op in ops:
            lst = []
            for d in op.deps:
                dop = ops[d]
                if dop.kind == "bar":
                    continue
                if dop.kind == "dma":
                    lst.append(d)
                elif dop.eng == op.eng:
                    if op.kind != "cmp" or op.eng != "pe" or not _PE_EXEMPT or (op.force and (op.eidx - dop.eidx) <= 3):
                        lst.append(d)
                        dop.need_sig = True
                else:
                    lst.append(d)
                    dop.need_sig = True
            need[op.idx] = lst
        cnt = {e: 0 for e in self.ENGS}
        for e in self.ENGS:
            for op in self.per_eng[e]:
                if op.kind == "cmp" and op.need_sig:
                    cnt[e] += 1
                    op.sig = cnt[e]
        with ExitStack() as st:
            csem = {}
            for e in self.ENGS:
                n = cnt[e] // SEM_LIM + 1
                csem[e] = [st.enter_context(nc.semaphore(f"c_{e}_{i}")) for i in range(n)]
            dsem = {}
            for e in self.ENGS:
                if self.ndma[e]:
                    dsem[e] = [st.enter_context(nc.semaphore(f"d_{e}_{i}")) for i in range(DMA_K)]
            block = st.enter_context(nc.Block())

            def run_engine(e, eng):
                seen_c = {x: 0 for x in self.ENGS}
                seen_d = {}
                for op in self.per_eng[e]:
                    if op.kind == "dma" and op.didx >= DMA_K:
                        key = (e, op.slot)
                        tgt = op.target - 16
                        if seen_d.get(key, 0) < tgt:
                            eng.wait_ge(dsem[e][op.slot], tgt)
                            seen_d[key] = tgt
                    for d in need[op.idx]:
                        dop = ops[d]
                        if dop.kind == "dma":
                            key = (dop.eng, dop.slot)
                            if seen_d.get(key, 0) < dop.target:
                                eng.wait_ge(dsem[dop.eng][dop.slot], dop.target)
                                seen_d[key] = dop.target
                        else:
                            if seen_c[dop.eng] < dop.sig:
                                s = dop.sig - 1
                                if s // SEM_LIM > (seen_c[dop.eng] - 1) // SEM_LIM and seen_c[dop.eng] > 0:
                                    pass
                                eng.wait_ge(csem[dop.eng][s // SEM_LIM], s % SEM_LIM + 1)
                                seen_c[dop.eng] = dop.sig
                    if op.kind == "bar":
                        continue
                    ins = op.fn(eng)
                    if op.kind == "dma":
                        ins.then_inc(dsem[e][op.slot], 16)
                    elif op.sig is not None:
                        s = op.sig - 1
                        ins.then_inc(csem[e][s // SEM_LIM], 1)
                if self.ndma[e]:
                    for slot in range(DMA_K):
                        last = None
                        for op in reversed(self.per_eng[e]):
                            if op.kind == "dma" and op.slot == slot:
                                last = op
                                break
                        if last is not None and seen_d.get((e, slot), 0) < last.target:
                            eng.wait_ge(dsem[e][slot], last.target)

            if self.per_eng["sp"]:
                @block.sync
                def _(eng):
                    run_engine("sp", eng)
            if self.per_eng["act"]:
                @block.scalar
                def _(eng):
                    run_engine("act", eng)
            if self.per_eng["pool"]:
                @block.gpsimd
                def _(eng):
                    run_engine("pool", eng)
            if self.per_eng["pe"]:
                @block.tensor
                def _(eng):
                    run_engine("pe", eng)
            if self.per_eng["dve"]:
                @block.vector
                def _(eng):
                    run_engine("dve", eng)


class Arena:
    def __init__(self, ap, cap):
        self.ap = ap
        self.cap = cap
        self.off = 0

    def mark(self):
        return self.off

    def release(self, m):
        self.off = m

    def _alloc(self, words):
        o = self.off
        self.off += words
        assert self.off <= self.cap, f"arena overflow {self.off} > {self.cap}"
        return o

    def f32(self, n):
        o = self._alloc(n)
        return self.ap[:, o:o + n]

    def bf16(self, n):
        w = (n + 1) // 2
        o = self._alloc(w)
        return self.ap[:, o:o + w].bitcast(BF16)


ARENA_WORDS = 47600


def pbcast(ap2d_row, n):
    return bass.AP(ap2d_row.tensor, ap2d_row.offset, [[0, 128], [1, n]])


class Builder:
    def __init__(self, dbg=None, stop=None):
        self.dbg = dbg
        self.stop = stop
        self.nc = bass.Bass("TRN2", target_bir_lowering=False)
        self.S = Sched(self.nc)

    def mm(self, out, lhsT, rhs, start, stop, R, W, serial=False):
        self.S.add("pe", lambda e: e.matmul(out, lhsT, rhs, start=start, stop=stop), R, W, serial=serial)

    def tr(self, out, in_, ident, R, W):
        self.S.add("pe", lambda e: e.transpose(out, in_, ident), R, W)

    def act(self, out, in_, func, R, W, **kw):
        self.S.add("act", lambda e: e.activation(out, in_, func, **kw), R, W)

    def cp(self, eng, out, in_, R, W):
        if eng == "act":
            self.S.add("act", lambda e: e.copy(out, in_), R, W)
        else:
            self.S.add(eng, lambda e: e.tensor_copy(out, in_), R, W)

    def tt(self, eng, out, in0, in1, op, R, W):
        self.S.add(eng, lambda e: e.tensor_tensor(out, in0, in1, op), R, W)

    def ts(self, eng, out, in0, s1, s2, op0, op1, R, W):
        if s2 is None:
            self.S.add(eng, lambda e: e.tensor_scalar(out, in0, s1, None, op0), R, W)
        else:
            self.S.add(eng, lambda e: e.tensor_scalar(out, in0, s1, s2, op0, op1), R, W)

    def stt(self, eng, out, in0, sc, in1, op0, op1, R, W):
        self.S.add(eng, lambda e: e.scalar_tensor_tensor(out, in0, sc, in1, op0, op1), R, W)

    def sumsq(self, junk, x, acc, R, W):
        self.S.add("dve", lambda e: e.scalar_tensor_tensor(junk, x, 1.0, x, ALU.mult, ALU.mult, accum_out=acc), R, W)

    def red(self, out, in_, op, R, W):
        self.S.add("dve", lambda e: e.tensor_reduce(out, in_, AX.X, op), R, W)

    def memset(self, eng, ap, val, W):
        self.S.add(eng, lambda e: e.memset(ap, val), (), W)

    def ld(self, out, in_, R, W, eng="sp", **kw):
        self.S.dma(eng, out, in_, R, W, **kw)

    def build(self):
        nc = self.nc
        S = self.S

        def din(name, shape, dt=F32):
            return nc.dram_tensor(name, list(shape), dt, kind="ExternalInput").ap()

        def dscr(name, shape, dt):
            return nc.dram_tensor(name, list(shape), dt).ap()

        I = {}
        I["x"] = din("x", [SEQ, D]); I["ctx"] = din("ctx", [CTX, D]); I["cc"] = din("cc", [2, D])
        I["ada_w"] = din("ada_w", [2, D, 6 * D]); I["ada_b"] = din("ada_b", [2, 6 * D])
        I["norm1_g"] = din("norm1_g", [2, D]); I["norm2_g"] = din("norm2_g", [2, D])
        I["w_in"] = din("w_in", [2, D, DIN]); I["w_out"] = din("w_out", [2, D, D])
        I["rdl"] = din("ret_decay_logit", [2, 2, 6]); I["gng"] = din("ret_gn_g", [2, 384])
        I["conv_w"] = din("conv_w", [2, 3, 256]); I["rpb"] = din("na_rpb", [2, 6, 15, 31])
        I["ffn_w_in"] = din("ffn_w_in", [1, D, 2 * DFF]); I["ffn_w_out"] = din("ffn_w_out", [1, DFF, D])
        I["rw"] = din("moe_router_w", [1, D, NEXP]); I["rb"] = din("moe_router_b", [1, NEXP])
        I["moe_w_in"] = din("moe_w_in", [1, NEXP, D, 2 * DFF]); I["moe_w_out"] = din("moe_w_out", [1, NEXP, DFF, D])
        I["final_g"] = din("final_g", [D]); I["sel"] = din("sel", [128, 2])
        I["ident_bf"] = din("ident_bf", [128, 128], BF16); I["jrev_bf"] = din("jrev_bf", [128, 128], BF16)
        I["ident_f"] = din("ident_f", [128, 128]); I["jrev_f"] = din("jrev_f", [128, 128])
        I["rope_c"] = din("rope_c", [NT * 128, 64]); I["rope_s"] = din("rope_s", [NT * 128, 64])
        I["pos4"] = din("pos4", [128, 4]); I["iota12"] = din("iota12", [128, 2, 128])
        I["dtab"] = din("dtab", [128, 4, 128]); I["blockmask"] = din("blockmask", [128, 128])
        I["na_cmask"] = din("na_cmask", [128, 64])
        self.I = I
        self.out = nc.dram_tensor("out", [2048, D], F32, kind="ExternalOutput").ap()
        if self.dbg:
            self.dbg_outs = {n: nc.dram_tensor(n, list(sh), dt_, kind="ExternalOutput").ap() for (n, sh, dt_) in self.dbg[1]}

        self.H = dscr("H", [NT * 128, D], F32)
        self.H1 = dscr("H1", [NT * 128, D], F32)
        self.MOD = dscr("MOD", [2, 6 * D], F32)
        self.TM = dscr("TM", [NT * 128, 1408], BF16)
        self.UU = dscr("UU", [NT * 128 + 3, 256], BF16)
        self.NV = dscr("NV", [NT * 128, 384], BF16)
        self.QT = dscr("QT", [NT, 128, 384], BF16)
        self.KT = dscr("KT", [NT, 128, 384], BF16)
        self.NQT = dscr("NQT", [NT, 128, 384], BF16)
        self.NKT = dscr("NKT", [3, 128, NT * 128], BF16)
        self.ST = dscr("ST", [NT, 2, 128, 384], BF16)
        self.PZ = dscr("PZ", [90, 127], F32)
        self.BE = dscr("BE", [4, 128, 6 * 576], F32)
        if self.dbg:
            self.YC = dscr("YC", [NT * 128, D], BF16)

        with ExitStack() as st:
            arena_t = st.enter_context(nc.sbuf_tensor("arena", [128, ARENA_WORDS], F32))
            pA_t = st.enter_context(nc.psum_tensor("pA", [128, 6, 512], F32))
            pT_t = st.enter_context(nc.psum_tensor("pT", [128, 2, 1024], BF16))
            self.ar = Arena(arena_t[:, :], ARENA_WORDS)
            self.pA = [pA_t[:, i, :] for i in range(6)]
            self.pT = [pT_t[:, i, :] for i in range(2)]
            self.pAn = [f"pA{i}" for i in range(6)]
            self.pTn = [f"pT{i}" for i in range(2)]
            self.program()
            S.emit()
        return nc

    def program(self):
        S = self.S
        ar = self.ar
        I = self.I
        self.ident = ar.bf16(128); self.jrev = ar.bf16(128)
        self.ld(self.ident, I["ident_bf"], (), ["ident"]); self.ld(self.jrev, I["jrev_bf"], (), ["jrev"])
        self.cB = ar.bf16(16)
        m0 = ar.mark()
        cT = ar.f32(16)
        self.ld(cT.rearrange("p (r k) -> p r k", r=2), I["cc"].rearrange("r (k p) -> p r k", p=128), (), ["cT"],
                allow_slow_non_contiguous=True)
        cA = ar.f32(16)
        self.act(cA, cT, AF.Silu, ["cT"], ["cA"])
        self.cp("dve", self.cB.rearrange("p (k r) -> p r k", r=2), cA.rearrange("p (r k) -> p r k", r=2), ["cA"], ["cB"])
        S.barrier()
        ar.release(m0)
        gmark = ar.mark()
        for l in range(DEPTH):
            ar.release(gmark)
            self.phase_mods(l)
            S.barrier(); ar.release(gmark)
            if self.stop == f"mods{l}":
                break
            self.phase_p1(l)
            S.barrier(); ar.release(gmark)
            if self.stop == f"p1_{l}":
                break
            self.phase_tables(l)
            self.phase_p2(l)
            S.barrier()
            if self.stop == f"p2_{l}":
                break
            self.phase_p3(l)
            S.barrier(); ar.release(gmark)
            if self.stop == f"p3_{l}":
                break
            if l == 0:
                self.phase_ffn(l, "ctx", [0, 1], I["ffn_w_in"][0], I["ffn_w_out"][0], None)
                S.barrier(); ar.release(gmark)
                self.phase_ffn(l, "lat", list(range(2, 18)), I["ffn_w_in"][0], I["ffn_w_out"][0], None)
                S.barrier(); ar.release(gmark)
                self.phase_ffn(l, "lat", list(range(18, 34)), I["ffn_w_in"][0], I["ffn_w_out"][0], None)
                S.barrier(); ar.release(gmark)
                if self.stop == "l0":
                    break
            else:
                self.phase_ffn(l, "moe", list(range(16)), I["moe_w_in"][0], I["moe_w_out"][0], 0)
                S.barrier(); ar.release(gmark)
        if self.dbg:
            S.barrier()
            self.dbg[0](self)

    def hsrc(self, l, ti):
        if l == 0:
            if ti < 2:
                return self.I["ctx"][ti * 128:(ti + 1) * 128, :]
            return self.I["x"][(ti - 2) * 128:(ti - 1) * 128, :]
        return self.H[ti * 128:(ti + 1) * 128, :]

    def modrow(self, kind, j):
        r = 0 if kind == "lat" else 1
        return pbcast(self.MOD[r:r + 1, j * D:(j + 1) * D], D)

    def phase_mods(self, l):
        ar = self.ar
        I = self.I
        wb = [ar.bf16(8 * 512), ar.bf16(8 * 512)]
        bb = [ar.f32(512), ar.f32(512)]
        ob = [ar.f32(512), ar.f32(512)]
        cBv = self.cB.rearrange("p (k r) -> p k r", r=2)
        for n in range(12):
            s = n % 2
            self.ld(wb[s].rearrange("p (k n) -> p k n", k=8),
                    I["ada_w"][l][:, n * 512:(n + 1) * 512].rearrange("(k p) n -> p k n", p=128),
                    (), [f"mw{s}"], eng="pool")
            src = I["ada_b"][l:l + 1, n * 512:(n + 1) * 512]
            self.ld(bb[s][0:2, :], bass.AP(src.tensor, src.offset, [[0, 2], [1, 512]]), (), [f"mb{s}"])
            pb = self.pA[s]
            for k in range(8):
                self.mm(pb[0:2, :], cBv[:, k, :], wb[s][:, k * 512:(k + 1) * 512], k == 0, k == 7,
                        [f"mw{s}", "cB"], [self.pAn[s]])
            self.tt("dve", ob[s][0:2, :], pb[0:2, :], bb[s][0:2, :], ALU.add, [self.pAn[s], f"mb{s}"], [f"mo{s}"])
            self.ld(self.MOD[:, n * 512:(n + 1) * 512], ob[s][0:2, :], [f"mo{s}"], ["MOD"])

    def phase_p1(self, l):
        ar = self.ar
        I = self.I
        S = self.S
        w = ar.bf16(8 * DIN)
        wv = w.rearrange("p (k n) -> p k n", k=8)
        for k in range(8):
            self.ld(wv[:, k, :], I["w_in"][l][k * 128:(k + 1) * 128, :], (), ["w_in"], eng="pool",
                    max_dma_last_dim=4096)
        g1n = ar.f32(D)
        self.ld(g1n, pbcast(I["norm1_g"][l:l + 1, :], D), (), ["g1n"])
        A1 = {}; SH = {}
        for kind in ("lat", "ctx"):
            if l == 1 and kind == "ctx" and False:
                continue
            A1[kind] = ar.f32(D); SH[kind] = ar.f32(D)
            self.ld(SH[kind], self.modrow(kind, 0), ["MOD"], [f"sh_{kind}"])
            self.ld(A1[kind], self.modrow(kind, 1), ["MOD"], [f"a1_{kind}"])
            self.stt("dve", A1[kind], A1[kind], 1.0, g1n, ALU.add, ALU.mult, [f"a1_{kind}", "g1n"], [f"a1_{kind}"])
        z = ar.bf16(256)
        self.memset("pool", z[0:1, :], 0.0, ["zrow"])
        for r in (0, 257, NT * 128 + 2):
            self.ld(self.UU[r:r + 1, :], z[0:1, :], ["zrow"], ["UU"])
        hb = [ar.f32(D), ar.f32(D), ar.f32(D)]
        junk = ar.f32(D)
        stat = ar.f32(NT * 2)
        self.memset("pool", stat, 0.0, ["stat0", "stat1"])
        tmp = ar.f32(D)
        ab = [ar.bf16(D), ar.bf16(D)]
        aT = [ar.bf16(D), ar.bf16(D)]
        pr = ar.f32(DIN)
        rc = [ar.f32(64) for _ in range(3)]; rs = [ar.f32(64) for _ in range(3)]
        t1 = ar.f32(768); t2 = ar.f32(768)
        qb = [ar.bf16(384), ar.bf16(384)]
        tmo = [ar.bf16(1408), ar.bf16(1408)]
        ub = [ar.bf16(256), ar.bf16(256)]
        nb = [ar.bf16(1152), ar.bf16(1152)]
        qkT = [ar.bf16(768), ar.bf16(768)]
        nnT = [ar.bf16(768), ar.bf16(768)]
        pr2 = [pr, ar.f32(DIN)]
        bankctr = [0]

        def loads_a(ti):
            s3 = ti % 3
            self.ld(hb[s3], self.hsrc(l, ti), ["H"], [f"hb{s3}"])
            self.ld(rc[s3], I["rope_c"][ti * 128:(ti + 1) * 128, :], (), [f"rc{s3}"])
            self.ld(rs[s3], I["rope_s"][ti * 128:(ti + 1) * 128, :], (), [f"rs{s3}"])

        def stage_a(ti):
            s = ti % 2
            s3 = ti % 3
            kind = "ctx" if ti < 2 else "lat"
            ss = stat[:, 2 * ti:2 * ti + 1]; rstd = stat[:, 2 * ti + 1:2 * ti + 2]
            self.sumsq(junk, hb[s3], ss, [f"hb{s3}"], ["junk", f"stat{s}"])
            self.act(rstd, ss, AF.Sqrt, [f"stat{s}"], [f"stat{s}"], bias=EPS, scale=1.0 / D)
            self.S.add("dve", lambda e, rstd=rstd: e.reciprocal(rstd, rstd), [f"stat{s}"], [f"stat{s}"])
            self.stt("dve", tmp, hb[s3], rstd, A1[kind], ALU.mult, ALU.mult, [f"hb{s3}", f"stat{s}", f"a1_{kind}"], ["tmp"])
            self.tt("pool", ab[s], tmp, SH[kind], ALU.add, ["tmp", f"sh_{kind}"], [f"ab{s}"])

        def stage_a_tr(ti):
            s = ti % 2
            tb = 0
            for k in range(8):
                self.tr(self.pT[tb][:, k * 128:(k + 1) * 128], ab[s][:, k * 128:(k + 1) * 128], self.ident,
                        [f"ab{s}", "ident"], [self.pTn[tb]])
            self.cp("act", aT[s], self.pT[tb], [self.pTn[tb]], [f"aT{s}"])

        def stage_b(ti, hooks):
            s = ti % 2
            p_ = pr2[s]
            for n in range(7):
                if n in hooks:
                    hooks[n]()
                cols = 512 if n < 6 else 384
                bk = bankctr[0] % 6
                bankctr[0] += 1
                for k in range(8):
                    self.mm(self.pA[bk][:, 0:cols], aT[s][:, k * 128:(k + 1) * 128], wv[:, k, n * 512:n * 512 + cols],
                            k == 0, k == 7, [f"aT{s}", "w_in"], [self.pAn[bk]])
                self.cp("act" if n % 2 == 0 else "dve", p_[:, n * 512:n * 512 + cols], self.pA[bk][:, 0:cols],
                        [self.pAn[bk]], [f"pr{s}_{n}"])

        def stage_c1(ti):
            s = ti % 2
            s3 = ti % 3
            p_ = pr2[s]
            P = lambda *ns: [f"pr{s}_{n}" for n in ns]
            qk3 = p_[:, 0:768].rearrange("p (h d) -> p h d", d=64)
            self.tt("dve", t1.rearrange("p (h d) -> p h d", d=64), qk3,
                    rc[s3].unsqueeze(1).broadcast_to([128, 12, 64]), ALU.mult, P(0, 1) + [f"rc{s3}"], ["t1"])
            qk5 = p_[:, 0:768].rearrange("p (h c x d) -> p h c x d", c=2, x=2, d=16)
            t25 = t2.rearrange("p (h c x d) -> p h c x d", c=2, x=2, d=16)
            rs4 = rs[s3].rearrange("p (c x d) -> p c x d", c=2, x=2)
            for xx in range(2):
                self.tt("pool", t25[:, :, :, xx, :], qk5[:, :, :, 1 - xx, :],
                        rs4[:, :, xx, :].unsqueeze(1).broadcast_to([128, 12, 2, 16]), ALU.mult,
                        P(0, 1) + [f"rs{s3}"], ["t2"])
            self.tt("dve", qb[s], t1[:, 0:384], t2[:, 0:384], ALU.add, ["t1", "t2"], [f"qb{s}"])
            self.tt("dve", tmo[s][:, 0:384], t1[:, 384:768], t2[:, 384:768], ALU.add, ["t1", "t2"], [f"tmo{s}"])
            self.cp("pool", tmo[s][:, 384:768], p_[:, 768:1152], P(1, 2), [f"tmo{s}"])
            self.act(tmo[s][:, 768:1152], p_[:, 1152:1536], AF.Silu, P(2), [f"tmo{s}"])
            self.cp("pool", tmo[s][:, 1152:1408], p_[:, 1536:1792], P(3), [f"tmo{s}"])
            self.tt("pool", ub[s], p_[:, 1792:2048], p_[:, 2048:2304], ALU.mult, P(3, 4), [f"ub{s}"])
            self.cp("dve", nb[s], p_[:, 2304:3456], P(4, 5, 6), [f"nb{s}"])
            self.ld(self.TM[ti * 128:(ti + 1) * 128, :], tmo[s], [f"tmo{s}"], ["TM"], eng=_P1ST)
            urow = 1 + ti * 128 if ti < 2 else 2 + ti * 128
            self.ld(self.UU[urow:urow + 128, :], ub[s], [f"ub{s}"], ["UU"], eng=_P1ST)
            self.ld(self.NV[ti * 128:(ti + 1) * 128, :], nb[s][:, 768:1152], [f"nb{s}"], ["NV"], eng=_P1ST)

        def stage_c2(ti):
            s = ti % 2
            tb = 1
            for g in range(3):
                self.tr(self.pT[tb][:, g * 128:(g + 1) * 128], qb[s][:, g * 128:(g + 1) * 128], self.ident,
                        [f"qb{s}", "ident"], [self.pTn[tb]])
                self.tr(self.pT[tb][:, 384 + g * 128:384 + (g + 1) * 128], tmo[s][:, g * 128:(g + 1) * 128], self.ident,
                        [f"tmo{s}", "ident"], [self.pTn[tb]])
            self.cp("act", qkT[s], self.pT[tb][:, 0:768], [self.pTn[tb]], [f"qkT{s}"])
            self.ld(self.QT[ti], qkT[s][:, 0:384], [f"qkT{s}"], ["QT"], eng=_P1ST)
            self.ld(self.KT[ti], qkT[s][:, 384:768], [f"qkT{s}"], ["KT"], eng=_P1ST)
            for g in range(3):
                self.tr(self.pT[tb][:, g * 128:(g + 1) * 128], nb[s][:, g * 128:(g + 1) * 128], self.jrev,
                        [f"nb{s}", "jrev"], [self.pTn[tb]])
                self.tr(self.pT[tb][:, 384 + g * 128:384 + (g + 1) * 128], nb[s][:, 384 + g * 128:384 + (g + 1) * 128],
                        self.ident, [f"nb{s}", "ident"], [self.pTn[tb]])
            self.cp("dve", nnT[s], self.pT[tb][:, 0:768], [self.pTn[tb]], [f"nnT{s}"])
            self.ld(self.NQT[ti], nnT[s][:, 0:384], [f"nnT{s}"], ["NQT"], eng=_P1ST)
            self.ld(self.NKT[:, :, ti * 128:(ti + 1) * 128].rearrange("g p t -> p g t"),
                    nnT[s][:, 384:768].rearrange("p (g t) -> p g t", g=3), [f"nnT{s}"], ["NKT"], eng=_P1ST)

        loads_a(0)
        loads_a(1)
        stage_a(0)
        stage_a_tr(0)
        for ti in range(NT):
            hooks = {}
            if ti + 2 < NT:
                loads_a(ti + 2)
            if ti + 1 < NT:
                stage_a(ti + 1)
                hooks[6] = (lambda t=ti + 1: stage_a_tr(t))
            if ti >= 1:
                hooks[3] = (lambda t=ti - 1: stage_c2(t))
            stage_b(ti, hooks)
            stage_c1(ti)
        stage_c2(NT - 1)

    def phase_tables(self, l):
        ar = self.ar
        I = self.I
        T = {}
        self.T = T
        lgc = ar.f32(12)
        src = I["rdl"][l]
        self.ld(lgc, bass.AP(src.tensor, src.offset, [[0, 128], [1, 12]]), (), ["lgc"])
        lgp = ar.f32(6)
        for half in range(2):
            self.ld(lgp[half * 64:(half + 1) * 64, :].rearrange("p (d g) -> p d g", d=2),
                    bass.AP(src.tensor, src.offset + half, [[0, 64], [6, 2], [2, 3]]), (), ["lgp"],
                    allow_slow_non_contiguous=True)
        for nm, t, n in (("lgc", lgc, 12), ("lgp", lgp, 6)):
            self.act(t, t, AF.Exp, [nm], [nm], scale=-1.0)
            self.act(t, t, AF.Ln, [nm], [nm], bias=1.0, scale=1.0)
            self.ts("dve", t, t, -1.0, None, ALU.mult, None, [nm], [nm])
        pos4 = ar.f32(4)
        self.ld(pos4, I["pos4"], (), ["pos4"])
        io = ar.f32(256)
        self.ld(io, I["iota12"].rearrange("p a b -> p (a b)"), (), ["io"])
        dt = ar.f32(512)
        self.ld(dt, I["dtab"].rearrange("p a b -> p (a b)"), (), ["dt"])
        T["bm"] = ar.f32(128)
        self.ld(T["bm"], I["blockmask"], (), ["bm"])
        e6 = ar.f32(12)
        self.act(e6[:, 0:6], lgc[:, 0:6], AF.Exp, ["lgc", "pos4"], ["e6"], scale=pos4[:, 1:2])
        self.act(e6[:, 6:12], lgc[:, 6:12], AF.Exp, ["lgc", "pos4"], ["e6"], scale=pos4[:, 0:1])
        T["kd"] = ar.f32(768)
        self.ts("dve", T["kd"].rearrange("p (h d) -> p h d", d=64), e6.unsqueeze(2).broadcast_to([128, 12, 64]),
                0.125, None, ALU.mult, None, ["e6"], ["kd"])
        T["dq"] = ar.f32(768)
        for d_ in range(2):
            for g in range(3):
                o = (d_ * 3 + g) * 128
                self.act(T["dq"][:, o:o + 128], io[:, d_ * 128:(d_ + 1) * 128], AF.Exp, ["lgp", "io"], ["dq"],
                         scale=lgp[:, d_ * 3 + g:d_ * 3 + g + 1])
        T["gam"] = ar.f32(6)
        self.act(T["gam"], lgp, AF.Exp, ["lgp"], ["gam"], scale=128.0)
        T["M"] = ar.f32(768)
        ef = ar.f32(128); eb = ar.f32(128)
        for h in range(6):
            self.act(ef, dt[:, 0:128], AF.Exp, ["lgc", "dt"], ["ef"], scale=lgc[:, h:h + 1])
            self.act(eb, dt[:, 128:256], AF.Exp, ["lgc", "dt"], ["eb"], scale=lgc[:, 6 + h:7 + h])
            self.tt("dve", ef, ef, dt[:, 256:384], ALU.mult, ["ef", "dt"], ["ef"])
            self.tt("dve", eb, eb, dt[:, 384:512], ALU.mult, ["eb", "dt"], ["eb"])
            self.tt("dve", ef, ef, eb, ALU.add, ["ef", "eb"], ["ef"])
            self.ts("dve", T["M"][:, h * 128:(h + 1) * 128], ef, 0.125, None, ALU.mult, None, ["ef"], ["M"])
        T["gng"] = ar.f32(384)
        self.ld(T["gng"], pbcast(I["gng"][l:l + 1, :], 384), (), ["gng"])
        T["cw"] = ar.f32(768)
        srcw = I["conv_w"][l]
        self.ld(T["cw"], bass.AP(srcw.tensor, srcw.offset, [[0, 128], [1, 768]]), (), ["cw"])
        m0 = ar.mark()
        T["bint"] = None
        pz = ar.f32(127)
        self.memset("pool", pz[0:90, :], 0.0, ["pz"])
        self.ld(pz[0:90, 48:79], I["rpb"][l].rearrange("h r c -> (h r) c"), (), ["pz"])
        self.ld(self.PZ, pz[0:90, :], ["pz"], ["PZ"])
        T["bint"] = ar.f32(6 * 576)
        m1 = ar.mark()
        tz = ar.f32(90 * 64)
        for half in range(2):
            self.ld(tz[half * 64:(half + 1) * 64, :].rearrange("p (r k) -> p r k", k=64),
                    bass.AP(self.PZ.tensor, 0, [[1, 64], [127, 90], [1, 64]]), ["PZ"], ["tz"])
        cm = ar.f32(64)
        self.ld(cm, I["na_cmask"], (), ["cm"])
        tz3 = tz.rearrange("p (r k) -> p r k", k=64)
        be = ar.f32(6 * 576)
        pats = [((3, 2), ((0, 8), (1, 9)), 9), ((7, 6), ((0, 8), (0, 8)), 8), ((5, 4), ((0, 8), (0, 8)), 8),
                ((3, 2), ((0, 8), (0, 8)), 8), ((1, 0), ((0, 8), (0, 8)), 8)]
        for pi, (offs, val, nrows) in enumerate(pats):
            dst = T["bint"] if pi == 0 else be
            dname = "bint" if pi == 0 else "be"
            d4 = dst.rearrange("p (h r k) -> p h r k", h=6, k=64)
            if pi == 0:
                self.memset("pool", dst, NEG, [dname])
            for h in range(6):
                for a in range(2):
                    lo, hi = val[a]
                    ps = slice(a * 64, (a + 1) * 64)
                    r0 = h * 15 + offs[a] + lo
                    self.tt("dve", d4[ps, h, lo:hi, :], tz3[ps, r0:r0 + (hi - lo), :],
                            cm[ps, :].unsqueeze(1).broadcast_to([64, hi - lo, 64]), ALU.add, ["tz", "cm"], [dname])
            if pi > 0:
                self.ld(self.BE[pi - 1], be, ["be"], ["BE"])
        self.S.barrier()
        ar.release(m1)

    def phase_p2(self, l):
        ar = self.ar
        T = self.T
        m0 = ar.mark()
        kvall = ar.bf16(NT * 768)
        for ti in range(NT):
            self.ld(kvall[:, ti * 768:(ti + 1) * 768], self.TM[ti * 128:(ti + 1) * 128, 0:768], ["TM"], [f"kv{ti}"])
        kd = [ar.bf16(384) for _ in range(2)]
        um = [ar.f32(384) for _ in range(2)]
        Sst = [ar.f32(384) for _ in range(2)]
        sb = [ar.bf16(384) for _ in range(4)]
        for d_ in range(2):
            self.memset("pool", Sst[d_], 0.0, [f"S{d_}"])
        order = [list(range(NT)), [1, 0] + list(range(NT - 1, 1, -1))]
        bmb = T["bm"].unsqueeze(1).broadcast_to([128, 3, 128])
        for step in range(NT):
            for d_ in range(2):
                ti = order[d_][step]
                s = (step * 2 + d_) % 4
                kvt = kvall[:, ti * 768:(ti + 1) * 768]
                self.cp("act", sb[s], Sst[d_], [f"S{d_}"], [f"sb{s}"])
                self.ld(self.ST[ti, d_], sb[s], [f"sb{s}"], ["ST"])
                self.tt("pool", kd[d_], kvt[:, 0:384], T["kd"][:, d_ * 384:(d_ + 1) * 384], ALU.mult,
                        [f"kv{ti}", "kd"], [f"kdb{d_}"])
                pb = self.pA[d_]
                for g in range(3):
                    self.mm(pb[:, g * 128:(g + 1) * 128], kd[d_][:, g * 128:(g + 1) * 128],
                            kvt[:, 384 + g * 128:384 + (g + 1) * 128], True, True,
                            [f"kdb{d_}", f"kv{ti}"], [self.pAn[d_]])
                self.tt("dve", um[d_].rearrange("p (g n) -> p g n", g=3), pb[:, 0:384].rearrange("p (g n) -> p g n", g=3),
                        bmb, ALU.mult, [self.pAn[d_], "bm"], [f"um{d_}"])
                for g in range(3):
                    self.stt("dve", Sst[d_][:, g * 128:(g + 1) * 128], Sst[d_][:, g * 128:(g + 1) * 128],
                             T["gam"][:, d_ * 3 + g:d_ * 3 + g + 1], um[d_][:, g * 128:(g + 1) * 128],
                             ALU.mult, ALU.add, [f"S{d_}", "gam", f"um{d_}"], [f"S{d_}"])
        ar.release(m0)

    def phase_p3(self, l):
        ar = self.ar
        I = self.I
        T = self.T
        tiles = list(range(NT)) if l == 0 else list(range(2, NT))
        wo = ar.bf16(8 * D)
        wov = wo.rearrange("p (k n) -> p k n", k=8)
        for k in range(8):
            self.ld(wov[:, k, :], I["w_out"][l][k * 128:(k + 1) * 128, :], (), ["w_out"], eng="pool")
        G1 = {}
        for kind in (("lat", "ctx") if l == 0 else ("lat",)):
            G1[kind] = ar.f32(D)
            self.ld(G1[kind], self.modrow(kind, 2), ["MOD"], [f"g1_{kind}"])
        nkc = ar.bf16(3 * 256)
        self.ld(nkc.rearrange("p (g t) -> p g t", g=3), self.NKT[:, :, 0:256].rearrange("g p t -> p g t"), ["NKT"], ["nkc"])
        nvc = ar.bf16(2 * 384)
        self.ld(nvc.rearrange("p (j c) -> p j c", j=2), self.NV[0:256, :].rearrange("(j p) c -> p j c", p=128), ["NV"], ["nvc"])
        bedge = ar.f32(6 * 576)
        jrf = ar.f32(128)
        self.ld(jrf, I["jrev_f"], (), ["jrf"])
        nbuf = 2
        qt = [ar.bf16(384) for _ in range(nbuf)]; kt = [ar.bf16(384) for _ in range(nbuf)]
        stf = [ar.bf16(384) for _ in range(nbuf)]; stb = [ar.bf16(384) for _ in range(nbuf)]
        vgc = [ar.bf16(1024) for _ in range(nbuf)]
        u3 = [ar.bf16(768) for _ in range(nbuf)]
        nqt = [ar.bf16(384) for _ in range(nbuf)]
        nkw = [ar.bf16(3 * 576) for _ in range(nbuf)]
        nvw = [ar.bf16(5 * 384) for _ in range(nbuf)]
        hb = [ar.f32(D) for _ in range(nbuf)]
        qf = ar.bf16(384); qbk = ar.bf16(384)
        A = ar.bf16(768)
        osb = ar.f32(384); osq = ar.f32(384)
        stt_ = ar.f32(48)
        ycat = [ar.bf16(D) for _ in range(2)]
        c1 = ar.f32(256); c2 = ar.f32(256)
        sc = [ar.f32(832) for _ in range(2)]
        pb_ = [ar.bf16(832) for _ in range(2)]
        pts = [ar.bf16(896) for _ in range(2)]
        nst = ar.f32(NT * 6 * 4)
        self.memset("pool", nst, 0.0, ["nst3"] + [f"nm{h}" for h in range(6)] + [f"nr{h}" for h in range(6)])
        yT = ar.bf16(D)
        h1 = [ar.f32(D) for _ in range(2)]
        wtmp = ar.f32(D)
        scale = 0.125

        def loads(ti):
            s = ti % nbuf
            R = {}
            self.ld(qt[s], self.QT[ti], ["QT"], [f"qt{s}"])
            self.ld(kt[s], self.KT[ti], ["KT"], [f"kt{s}"])
            self.ld(stf[s], self.ST[ti, 0], ["ST"], [f"stf{s}"])
            self.ld(stb[s], self.ST[ti, 1], ["ST"], [f"stb{s}"])
            self.ld(vgc[s], self.TM[ti * 128:(ti + 1) * 128, 384:1408], ["TM"], [f"vgc{s}"])
            urow = 1 + ti * 128 if ti < 2 else 2 + ti * 128
            self.ld(u3[s].rearrange("p (j c) -> p j c", j=3),
                    bass.AP(self.UU.tensor, (urow - 1) * 256, [[256, 128], [256, 3], [1, 256]]), ["UU"], [f"u3{s}"])
            self.ld(nqt[s], self.NQT[ti], ["NQT"], [f"nqt{s}"])
            self.ld(hb[s], self.hsrc(l, ti), ["H"], [f"h3{s}"])
            if ti >= 2:
                c = ti - 2
                if c <= 1:
                    R0, nrows, pat = 0, 8, 1 + c
                elif c >= 30:
                    R0, nrows, pat = 56, 8, 3 + (c - 30)
                else:
                    R0, nrows, pat = 2 * c - 4, 9, 0
                t0 = 256 + R0 * 64
                nk = nrows * 64
                self.ld(nkw[s].rearrange("p (g t) -> p g t", g=3)[:, :, 0:nk],
                        self.NKT[:, :, t0:t0 + nk].rearrange("g p t -> p g t"), ["NKT"], [f"nkw{s}"])
                self.ld(nvw[s].rearrange("p (j c) -> p j c", j=5)[:, 0:4, :],
                        self.NV[t0:t0 + 512, :].rearrange("(j p) c -> p j c", p=128), ["NV"], [f"nvw{s}"])
                if nrows == 9:
                    self.ld(nvw[s][0:64, 4 * 384:5 * 384], self.NV[t0 + 512:t0 + 576, :], ["NV"], [f"nvw{s}"])
                return (nrows, pat)
            return (0, -1)

        X, Y, Z = 0, 1, 2

        def ret_part(ti):
            s = ti % nbuf
            dq = T["dq"]
            self.tt("pool", qf, qt[s], dq[:, 0:384], ALU.mult, [f"qt{s}", "dq"], ["qf"])
            self.tt("pool", qbk, qt[s], dq[:, 384:768], ALU.mult, [f"qt{s}", "dq"], ["qbk"])
            X, Y, Z = 0, 1, 2
            for h in range(6):
                g, hf = h // 2, h % 2
                psl = slice(hf * 64, (hf + 1) * 64)
                bank, col = (X, h * 128) if h < 4 else (Y, (h - 4) * 128)
                self.mm(self.pA[bank][:, col:col + 128], kt[s][psl, g * 128:(g + 1) * 128],
                        qt[s][psl, g * 128:(g + 1) * 128], True, True, [f"kt{s}", f"qt{s}"], [self.pAn[bank]], serial=True)
            self.tt("dve", A[:, 0:512], self.pA[X][:, 0:512], T["M"][:, 0:512], ALU.mult, [self.pAn[X], "M"], ["A"])
            self.tt("dve", A[:, 512:768], self.pA[Y][:, 0:256], T["M"][:, 512:768], ALU.mult, [self.pAn[Y], "M"], ["A"])
            for g in range(3):
                cs = slice(g * 128, (g + 1) * 128)
                self.mm(self.pA[Z][:, cs], qf[:, cs], stf[s][:, cs], True, False, ["qf", f"stf{s}"], [self.pAn[Z]])
                self.mm(self.pA[Z][:, cs], qbk[:, cs], stb[s][:, cs], False, False, ["qbk", f"stb{s}"], [self.pAn[Z]])
                for h in (2 * g, 2 * g + 1):
                    self.mm(self.pA[Z][:, h * 64:(h + 1) * 64], A[:, h * 128:(h + 1) * 128], vgc[s][:, h * 64:(h + 1) * 64],
                            False, h == 2 * g + 1, ["A", f"vgc{s}"], [self.pAn[Z]])

        info = {}
        info[tiles[0]] = loads(tiles[0])
        _hoist = _os.environ.get("P3_HOIST", "0") == "1"
        _hoist2 = _os.environ.get("P3_HOIST", "2") == "2"
        if _hoist or _hoist2:
            ret_part(tiles[0])
        for ix, ti in enumerate(tiles):
            s = ti % nbuf
            ys = ix % 2
            kind = "ctx" if ti < 2 else "lat"
            if ix + 1 < len(tiles):
                info[tiles[ix + 1]] = loads(tiles[ix + 1])
            nrows, pat = info[ti]
            if pat > 0:
                self.ld(bedge, self.BE[pat - 1], ["BE"], ["bedge"])
            if not (_hoist or _hoist2):
                ret_part(ti)
            nk = nrows * 64
            ln = 256 + nk
            nch = (ln + 127) // 128
            bias = T["bint"] if pat == 0 else bedge
            bname = "bint" if pat == 0 else "bedge"
            PA, PB, PC = 3, 4, 5
            o = ti * 24
            nkc3 = nkc.rearrange("p (g t) -> p g t", g=3)
            nkw3 = nkw[s].rearrange("p (g t) -> p g t", g=3)
            nvc3 = nvc.rearrange("p (j c) -> p j c", j=2)
            nvw3 = nvw[s].rearrange("p (j c) -> p j c", j=5)

            def na_s1(h):
                g, hf = h // 2, h % 2
                psl = slice(hf * 64, (hf + 1) * 64)
                hs = h % 2
                q = nqt[s][psl, g * 128:(g + 1) * 128]
                self.mm(self.pA[PB][:, 0:256], q, nkc3[psl, g, :], True, True, [f"nqt{s}", "nkc"], [self.pAn[PB]], serial=True)
                if nk:
                    self.mm(self.pA[PB][:, 256:512], q, nkw3[psl, g, 0:256], True, True, [f"nqt{s}", f"nkw{s}"], [self.pAn[PB]],
                            serial=True)
                    self.mm(self.pA[PA][:, 0:nk - 256], q, nkw3[psl, g, 256:nk], True, True, [f"nqt{s}", f"nkw{s}"], [self.pAn[PA]],
                            serial=True)
                self.ts("dve", sc[hs][:, 0:256], self.pA[PB][:, 0:256], scale, None, ALU.mult, None,
                        [self.pAn[PB]], [f"sc{hs}"])
                if nk:
                    self.stt("dve", sc[hs][:, 256:512], self.pA[PB][:, 256:512], scale, bias[:, h * 576:h * 576 + 256],
                             ALU.mult, ALU.add, [self.pAn[PB], bname], [f"sc{hs}"])
                    self.stt("dve", sc[hs][:, 512:ln], self.pA[PA][:, 0:nk - 256], scale, bias[:, h * 576 + 256:h * 576 + nk],
                             ALU.mult, ALU.add, [self.pAn[PA], bname], [f"sc{hs}"])
                mx = nst[:, o + h:o + h + 1]; nmx = nst[:, o + 6 + h:o + 7 + h]
                self.red(mx, sc[hs][:, 0:ln], ALU.max, [f"sc{hs}"], [f"nm{h}"])
                self.ts("dve", nmx, mx, -1.0, None, ALU.mult, None, [f"nm{h}"], [f"nm{h}"])

            def na_s2(h):
                hs = h % 2
                nmx = nst[:, o + 6 + h:o + 7 + h]; rsum = nst[:, o + 12 + h:o + 13 + h]
                self.act(pb_[hs][:, 0:ln], sc[hs][:, 0:ln], AF.Exp, [f"sc{hs}", f"nm{h}"], [f"pb{hs}", f"nr{h}"], bias=nmx, scale=1.0,
                         accum_out=rsum)
                tb = hs
                for ch in range(nch):
                    w_ = min(128, ln - ch * 128)
                    self.tr(self.pT[tb][0:w_, ch * 128:(ch + 1) * 128], pb_[hs][:, ch * 128:ch * 128 + w_], self.jrev,
                            [f"pb{hs}", "jrev"], [self.pTn[tb]])
                nfull = ln // 128
                self.cp("act", pts[hs][:, 0:nfull * 128], self.pT[tb][:, 0:nfull * 128], [self.pTn[tb]], [f"pts{hs}"])
                if nch > nfull:
                    self.cp("act", pts[hs][0:64, nfull * 128:nch * 128], self.pT[tb][0:64, nfull * 128:nch * 128],
                            [self.pTn[tb]], [f"pts{hs}"])

            def na_s3(h):
                hs = h % 2
                oc = slice(h * 64, (h + 1) * 64)
                for ch in range(nch):
                    w_ = min(128, ln - ch * 128)
                    rhs = nvc3[0:w_, ch, oc] if ch < 2 else nvw3[0:w_, ch - 2, oc]
                    self.mm(self.pA[PC][:, oc], pts[hs][0:w_, ch * 128:(ch + 1) * 128], rhs, ch == 0, ch == nch - 1,
                            [f"pts{hs}", "nvc", f"nvw{s}"], [self.pAn[PC]], serial=(w_ < 128))

            na_s1(0)
            self.cp("act", osb, self.pA[Z][:, 0:384], [self.pAn[Z]], ["osb"])
            self.act(osq, osb, AF.Square, ["osb"], ["osq"])
            o3 = osb.rearrange("p (h d) -> p h d", d=64)
            self.red(stt_[:, 0:6], o3, ALU.add, ["osb"], ["st_a"])
            self.red(stt_[:, 6:12], osq.rearrange("p (h d) -> p h d", d=64), ALU.add, ["osq"], ["st_b"])
            self.ts("dve", stt_[:, 12:18], stt_[:, 0:6], 1.0 / 64, None, ALU.mult, None, ["st_a"], ["st_c"])
            self.tt("dve", stt_[:, 18:24], stt_[:, 12:18], stt_[:, 12:18], ALU.mult, ["st_c"], ["st_d"])
            self.stt("dve", stt_[:, 24:30], stt_[:, 6:12], 1.0 / 64, stt_[:, 18:24], ALU.mult, ALU.subtract,
                     ["st_b", "st_d"], ["st_e"])
            self.act(stt_[:, 30:36], stt_[:, 24:30], AF.Sqrt, ["st_e"], ["st_f"], bias=EPS, scale=1.0)
            self.S.add("dve", lambda e: e.reciprocal(stt_[:, 36:42], stt_[:, 30:36]), ["st_f"], ["st_g"])
            self.tt("dve", o3, o3, stt_[:, 12:18].unsqueeze(2).broadcast_to([128, 6, 64]), ALU.subtract,
                    ["osb", "st_c"], ["osb"])
            self.tt("dve", o3, o3, stt_[:, 36:42].unsqueeze(2).broadcast_to([128, 6, 64]), ALU.mult,
                    ["osb", "st_g"], ["osb"])
            self.tt("pool", osb, osb, T["gng"], ALU.mult, ["osb", "gng"], ["osb"])
            self.tt("pool", ycat[ys][:, 0:384], osb, vgc[s][:, 384:768], ALU.mult, ["osb", f"vgc{s}"], [f"ycat{ys}"])
            cw = T["cw"]
            self.tt("pool", c1, u3[s][:, 0:256], cw[:, 0:256], ALU.mult, [f"u3{s}", "cw"], ["c1"])
            self.tt("pool", c2, u3[s][:, 256:512], cw[:, 256:512], ALU.mult, [f"u3{s}", "cw"], ["c2"])
            self.tt("pool", c1, c1, c2, ALU.add, ["c1", "c2"], ["c1"])
            self.tt("pool", c2, u3[s][:, 512:768], cw[:, 512:768], ALU.mult, [f"u3{s}", "cw"], ["c2"])
            self.tt("pool", c1, c1, c2, ALU.add, ["c1", "c2"], ["c1"])
            self.tt("pool", ycat[ys][:, 384:640], c1, vgc[s][:, 768:1024], ALU.mult, ["c1", f"vgc{s}"], [f"ycat{ys}"])
            na_s1(1)
            na_s2(0)
            for h in range(6):
                if h + 2 < 6:
                    na_s1(h + 2)
                if h + 1 < 6:
                    na_s2(h + 1)
                na_s3(h)
            if _hoist2 and ix + 1 < len(tiles):
                ret_part(tiles[ix + 1])
            self.mm(self.pA[PC][:, 384:390], jrf, nst[:, o + 12:o + 18], True, True, ["jrf"] + [f"nr{h}" for h in range(6)],
                    [self.pAn[PC]])
            self.S.add("dve", lambda e, o=o: e.reciprocal(nst[:, o + 18:o + 24], self.pA[PC][:, 384:390]), [self.pAn[PC]], ["nst3"])
            self.tt("dve", ycat[ys][:, 640:1024].rearrange("p (h d) -> p h d", d=64),
                    self.pA[PC][:, 0:384].rearrange("p (h d) -> p h d", d=64),
                    nst[:, o + 18:o + 24].unsqueeze(2).broadcast_to([128, 6, 64]), ALU.mult,
                    [self.pAn[PC], "nst3"], [f"ycat{ys}"])
            if self.dbg:
                self.ld(self.YC[ti * 128:(ti + 1) * 128, :], ycat[ys], [f"ycat{ys}"], ["YC"])
            if _hoist and ix + 1 < len(tiles):
                ret_part(tiles[ix + 1])
            tb = 0
            for k in range(8):
                self.tr(self.pT[tb][:, k * 128:(k + 1) * 128], ycat[ys][:, k * 128:(k + 1) * 128], self.ident,
                        [f"ycat{ys}", "ident"], [self.pTn[tb]])
            self.cp("act", yT, self.pT[tb], [self.pTn[tb]], ["yT"])
            for n in range(2):
                bk = 3 + n
                for k in range(8):
                    self.mm(self.pA[bk], yT[:, k * 128:(k + 1) * 128], wov[:, k, n * 512:(n + 1) * 512], k == 0, k == 7,
                            ["yT", "w_out"], [self.pAn[bk]])
                self.tt("dve", wtmp[:, n * 512:(n + 1) * 512], self.pA[bk], G1[kind][:, n * 512:(n + 1) * 512], ALU.mult,
                        [self.pAn[bk], f"g1_{kind}"], ["wtmp"])
            self.tt("pool", h1[ys], wtmp, hb[s], ALU.add, ["wtmp", f"h3{s}"], [f"h1_{ys}"])
            self.ld(self.H1[ti * 128:(ti + 1) * 128, :], h1[ys], [f"h1_{ys}"], ["H1"])

    def phase_ffn(self, l, kind, tiles, w1, w2, moe):
        ar = self.ar
        I = self.I
        S = self.S
        nt = len(tiles)
        ntok = nt * 128
        mk = "ctx" if kind == "ctx" else "lat"
        last = kind == "moe"
        SH2 = ar.f32(D); A2 = ar.f32(D); G2 = ar.f32(D)
        a2T = ar.bf16(8 * ntok)
        a2T_off = ar.off - 4 * ntok
        a2Tv = a2T.rearrange("p (k t) -> p k t", k=8)
        yacc = ar.f32(nt * D)
        st = ar.f32(nt * 4)
        self.memset("pool", st, 0.0, ["st0", "st1"])
        self.memset("pool", yacc, 0.0, ["yacc"])
        idf = ar.f32(128)
        self.ld(idf, I["ident_f"], (), ["idf"])
        if last:
            sel = ar.f32(2)
            rwt = ar.f32(64)
            rbt = ar.f32(8)
            self.ld(sel, I["sel"], (), ["sel"])
            self.ld(rwt.rearrange("p (k e) -> p k e", k=8), I["rw"][0].rearrange("(k p) e -> p k e", p=128), (), ["rwt"])
            self.ld(rbt, pbcast(I["rb"][0:1, :], 8), (), ["rbt"])
            gates = ar.f32(nt * 8)
            rt = ar.f32(64)
        NFS = 4
        supers = [(f0, min(NFS, 22 - f0)) for f0 in range(0, 22, NFS)]
        hid_off = ar.off
        hid = [ar.bf16(NFS * 512) for _ in range(2)]
        sgb = [ar.bf16(512) for _ in range(2)]
        w1b = [None, None]; w2b = [None, None]
        w1b[0] = ar.bf16(8 * 2 * NFS * 128); w2b[0] = ar.bf16(NFS * D)
        alias_off = ar.off
        w1b[1] = ar.bf16(8 * 2 * NFS * 128); w2b[1] = ar.bf16(NFS * D)
        base = ar.ap

        def f32at(off, n=D):
            return base[:, off:off + n]

        ha = [f32at(alias_off), f32at(alias_off + D)]
        hb2 = [f32at(alias_off + 2 * D), f32at(alias_off + 3 * D)]
        a32 = [f32at(alias_off + 4 * D), f32at(alias_off + 5 * D)]
        a32T = f32at(hid_off)
        assert alias_off + 6 * D <= ar.off and hid_off + D <= alias_off

        self.ld(hb2[0], pbcast(I["norm2_g"][l:l + 1, :], D), (), ["hb20"])
        self.ld(SH2, self.modrow(mk, 3), ["MOD"], ["sh2"])
        self.ld(A2, self.modrow(mk, 4), ["MOD"], ["a2m"])
        self.ld(G2, self.modrow(mk, 5), ["MOD"], ["g2m"])
        self.stt("dve", A2, A2, 1.0, hb2[0], ALU.add, ALU.mult, ["a2m", "hb20"], ["a2m"])

        experts = list(range(NEXP)) if moe is not None else [None]
        wlist = [(e, f0, nf) for e in experts for (f0, nf) in supers]

        def load_w(i):
            e, f0, nf = wlist[i]
            ws = i % 2
            W1 = w1 if e is None else w1[e]
            W2 = w2 if e is None else w2[e]
            w1v = w1b[ws].rearrange("p (k u c) -> p k u c", k=8, u=2)
            for u in range(2):
                c0 = u * DFF + f0 * 128
                self.ld(w1v[:, :, u, 0:nf * 128], W1[:, c0:c0 + nf * 128].rearrange("(k p) c -> p k c", p=128),
                        (), [f"w1b{ws}"], eng="pool")
            w2v = w2b[ws].rearrange("p (f n) -> p f n", f=NFS)
            self.ld(w2v[:, 0:nf, :], W2[f0 * 128:(f0 + nf) * 128, :].rearrange("(f p) n -> p f n", p=128),
                    (), [f"w2b{ws}"], eng="pool")

        load_w(0)

        def load_h(ti, p, ha_, hb_):
            if not last:
                self.ld(ha_[p], self.H1[ti * 128:(ti + 1) * 128, :], ["H1"], [f"ha{p}"])
            else:
                r0 = (2 + ti) * 128
                r1 = (2 + 16 + ti) * 128
                self.ld(ha_[p], self.H1[r0:r0 + 128, :], ["H1"], [f"ha{p}"])
                self.ld(hb_[p], self.H1[r1:r1 + 128, :], ["H1"], [f"hb2{p}"])
                self.ts("dve", ha_[p], ha_[p], sel[:, 0:1], None, ALU.mult, None, [f"ha{p}", "sel"], [f"ha{p}"])
                self.stt("dve", ha_[p], hb_[p], sel[:, 1:2], ha_[p], ALU.mult, ALU.add, [f"hb2{p}", "sel", f"ha{p}"], [f"ha{p}"])

        load_h(tiles[0], 0, ha, hb2)
        for j, ti in enumerate(tiles):
            p = j % 2
            if j + 1 < nt:
                load_h(tiles[j + 1], (j + 1) % 2, ha, hb2)
            ss = st[:, 4 * j:4 * j + 1]; rstd = st[:, 4 * j + 1:4 * j + 2]
            self.sumsq(a32[p], ha[p], ss, [f"ha{p}"], [f"a32{p}", f"st{p}"])
            self.act(rstd, ss, AF.Sqrt, [f"st{p}"], [f"st{p}"], bias=EPS, scale=1.0 / D)
            self.S.add("dve", lambda e, rstd=rstd: e.reciprocal(rstd, rstd), [f"st{p}"], [f"st{p}"])
            self.stt("dve", a32[p], ha[p], rstd, A2, ALU.mult, ALU.mult, [f"ha{p}", f"st{p}", "a2m"], [f"a32{p}"])
            self.tt("pool", a32[p], a32[p], SH2, ALU.add, [f"a32{p}", "sh2"], [f"a32{p}"])
            for k in range(8):
                bk = 2 * p + k // 4
                self.tr(self.pA[bk][:, (k % 4) * 128:(k % 4 + 1) * 128], a32[p][:, k * 128:(k + 1) * 128], idf,
                        [f"a32{p}", "idf"], [self.pAn[bk]])
            for b2 in range(2):
                bk = 2 * p + b2
                if last:
                    self.cp("dve", a32T[:, b2 * 512:(b2 + 1) * 512], self.pA[bk], [self.pAn[bk]], ["a32T"])
                    self.cp("act", a2Tv[:, 4 * b2:4 * b2 + 4, j * 128:(j + 1) * 128],
                            a32T[:, b2 * 512:(b2 + 1) * 512].rearrange("p (k t) -> p k t", k=4), ["a32T"], ["a2T"])
                else:
                    self.cp("act", a2Tv[:, 4 * b2:4 * b2 + 4, j * 128:(j + 1) * 128],
                            self.pA[bk].rearrange("p (k t) -> p k t", k=4), [self.pAn[bk]], ["a2T"])
            if last:
                rw3 = rwt.rearrange("p (k e) -> p k e", k=8)
                lg = rt[:, 0:8]; eq1 = rt[:, 8:16]; lg2 = rt[:, 16:24]; eq2 = rt[:, 24:32]
                m1 = rt[:, 32:33]; m2 = rt[:, 33:34]; dd = rt[:, 34:35]; ee = rt[:, 35:36]; w1_ = rt[:, 36:37]; w2_ = rt[:, 37:38]
                for k in range(8):
                    self.mm(self.pA[4][:, 0:8], a32T[:, k * 128:(k + 1) * 128], rw3[:, k, :], k == 0, k == 7,
                            ["a32T", "rwt"], [self.pAn[4]])
                self.tt("dve", lg, self.pA[4][:, 0:8], rbt, ALU.add, [self.pAn[4], "rbt"], ["rt0"])
                self.red(m1, lg, ALU.max, ["rt0"], ["rt1"])
                self.ts("dve", eq1, lg, m1, None, ALU.is_equal, None, ["rt0", "rt1"], ["rt2"])
                self.stt("dve", lg2, eq1, NEG, lg, ALU.mult, ALU.add, ["rt2", "rt0"], ["rt3"])
                self.red(m2, lg2, ALU.max, ["rt3"], ["rt4"])
                self.ts("dve", eq2, lg2, m2, None, ALU.is_equal, None, ["rt3", "rt4"], ["rt5"])
                self.tt("dve", dd, m2, m1, ALU.subtract, ["rt4", "rt1"], ["rt6"])
                self.act(ee, dd, AF.Exp, ["rt6"], ["rt7"])
                self.ts("dve", w1_, ee, 1.0, None, ALU.add, None, ["rt7"], ["rt8"])
                self.S.add("dve", lambda e, w1_=w1_: e.reciprocal(w1_, w1_), ["rt8"], ["rt8"])
                self.tt("dve", w2_, ee, w1_, ALU.mult, ["rt7", "rt8"], ["rt9"])
                gj = gates[:, j * 8:(j + 1) * 8]
                self.ts("dve", gj, eq1, w1_, None, ALU.mult, None, ["rt2", "rt8"], ["gates"])
                self.stt("dve", gj, eq2, w2_, gj, ALU.mult, ALU.add, ["rt5", "rt9", "gates"], ["gates"])
        S.barrier()
        groups = [list(range(g0, min(g0 + 4, nt))) for g0 in range(0, nt, 4)]
        fcnt = 0
        ycnt = 0
        gcnt = 0
        for i, (e, f0, nf) in enumerate(wlist):
            ws = i % 2
            if i + 1 < len(wlist):
                load_w(i + 1)
            w1v = w1b[ws].rearrange("p (k u c) -> p k u c", k=8, u=2)
            w2v = w2b[ws].rearrange("p (f n) -> p f n", f=NFS)
            for grp in groups:
                hs = gcnt % 2
                gcnt += 1
                t0 = grp[0] * 128
                ng = len(grp) * 128
                hv = hid[hs].rearrange("p (f t) -> p f t", f=NFS)
                for fi in range(nf):
                    b0 = 2 * (fcnt % 2)
                    sgs = fcnt % 2
                    fcnt += 1
                    for u in range(2):
                        for k in range(8):
                            self.mm(self.pA[b0 + u][:, 0:ng], w1v[:, k, u, fi * 128:(fi + 1) * 128], a2Tv[:, k, t0:t0 + ng],
                                    k == 0, k == 7, [f"w1b{ws}", "a2T"], [self.pAn[b0 + u]])
                    self.act(sgb[sgs][:, 0:ng], self.pA[b0][:, 0:ng], AF.Silu, [self.pAn[b0]], [f"sgb{sgs}"])
                    self.tt("dve", hv[:, fi, 0:ng], sgb[sgs][:, 0:ng], self.pA[b0 + 1][:, 0:ng], ALU.mult,
                            [f"sgb{sgs}", self.pAn[b0 + 1]], [f"hid{hs}"])
                for jj, j in enumerate(grp):
                    for oh in range(2):
                        bk = 4 + ycnt % 2
                        ycnt += 1
                        for fi in range(nf):
                            self.mm(self.pA[bk], hv[:, fi, jj * 128:(jj + 1) * 128], w2v[:, fi, oh * 512:(oh + 1) * 512],
                                    fi == 0, fi == nf - 1, [f"hid{hs}", f"w2b{ws}"], [self.pAn[bk]])
                        ya = yacc[:, j * D + oh * 512:j * D + (oh + 1) * 512]
                        if e is None:
                            self.tt("dve", ya, self.pA[bk], ya, ALU.add, [self.pAn[bk], "yacc"], ["yacc"])
                        else:
                            self.stt("dve", ya, self.pA[bk], gates[:, j * 8 + e:j * 8 + e + 1], ya, ALU.mult, ALU.add,
                                     [self.pAn[bk], "gates", "yacc"], ["yacc"])
        S.barrier()
        cha = [f32at(a2T_off), f32at(a2T_off + D)]
        chb = [f32at(a2T_off + 2 * D), f32at(a2T_off + 3 * D)]
        cr = [f32at(a2T_off + 4 * D), f32at(a2T_off + 5 * D)]
        co = [f32at(a2T_off + 6 * D), f32at(a2T_off + 7 * D)]
        assert 8 * D <= 4 * ntok or nt < 16
        if nt < 16:
            cha = ha; chb = hb2; cr = a32
            co = [f32at(alias_off - 3 * D), f32at(alias_off - 2 * D)]
        if last:
            self.ld(SH2, pbcast(I["final_g"].rearrange("(o d) -> o d", o=1), D), (), ["sh2"])
        load_h(tiles[0], 0, cha, chb)
        for j, ti in enumerate(tiles):
            p = j % 2
            if j + 1 < nt:
                load_h(tiles[j + 1], (j + 1) % 2, cha, chb)
            self.tt("pool", cr[p], yacc[:, j * D:(j + 1) * D], G2, ALU.mult, ["yacc", "g2m"], [f"cr{p}"])
            self.tt("dve", cr[p], cr[p], cha[p], ALU.add, [f"cr{p}", f"ha{p}"], [f"cr{p}"])
            if not last:
                self.ld(self.H[ti * 128:(ti + 1) * 128, :], cr[p], [f"cr{p}"], ["H"])
            else:
                ss = st[:, 4 * j + 2:4 * j + 3]; rstd = st[:, 4 * j + 3:4 * j + 4]
                self.sumsq(co[p], cr[p], ss, [f"cr{p}"], [f"co{p}", f"st{p}"])
                self.act(rstd, ss, AF.Sqrt, [f"st{p}"], [f"st{p}"], bias=EPS, scale=1.0 / D)
                self.S.add("dve", lambda e, rstd=rstd: e.reciprocal(rstd, rstd), [f"st{p}"], [f"st{p}"])
                self.stt("dve", co[p], cr[p], rstd, SH2, ALU.mult, ALU.mult, [f"cr{p}", f"st{p}", "sh2"], [f"co{p}"])
                self.ld(self.out[j * 128:(j + 1) * 128, :], co[p], [f"co{p}"], ["OUT"])


def _consts():
    bf = ml_dtypes.bfloat16
    C = {}
    C["ident_bf"] = np.eye(128, dtype=np.float32).astype(bf)
    J = np.zeros((128, 128), np.float32)
    for n in range(128):
        J[(n // 64) * 64 + 63 - n % 64, n] = 1.0
    C["jrev_bf"] = J.astype(bf)
    C["jrev_f"] = J.copy()
    C["ident_f"] = np.eye(128, dtype=np.float32)
    t = np.arange(SEQ)
    row = (t // 64).astype(np.float32)
    col = (t % 64).astype(np.float32)
    inv = (np.float32(10000.0) ** (-np.arange(16, dtype=np.float32) / np.float32(16))).astype(np.float32)
    ar_ = (row[:, None] * inv).astype(np.float32)
    ac_ = (col[:, None] * inv).astype(np.float32)
    cr, sr, cc_, sc_ = np.cos(ar_), np.sin(ar_), np.cos(ac_), np.sin(ac_)
    rc = np.concatenate([cr, cr, cc_, cc_], 1).astype(np.float32)
    rs = np.concatenate([-sr, sr, -sc_, sc_], 1).astype(np.float32)
    C["rope_c"] = np.concatenate([np.ones((CTX, 64), np.float32), rc], 0)
    C["rope_s"] = np.concatenate([np.zeros((CTX, 64), np.float32), rs], 0)
    p = np.arange(128, dtype=np.float32)
    C["pos4"] = np.stack([p, 127 - p, p + 1, 128 - p], 1).astype(np.float32)
    i = np.arange(128, dtype=np.float32)
    C["iota12"] = np.broadcast_to(np.stack([i + 1, 128 - i], 0)[None], (128, 2, 128)).astype(np.float32).copy()
    jj = p[:, None]
    ii = i[None, :]
    C["dtab"] = np.stack([np.maximum(ii - jj, 0), np.maximum(jj - ii, 0), (ii >= jj).astype(np.float32),
                          (jj >= ii).astype(np.float32)], 1).astype(np.float32).copy()
    hp = (np.arange(128) < 64)
    C["blockmask"] = (hp[:, None] == hp[None, :]).astype(np.float32)
    cm = np.full((128, 64), NEG, np.float32)
    for pp in range(128):
        qc = 63 - (pp % 64)
        c0 = min(max(qc - 8, 0), 48)
        cm[pp, c0:c0 + 16] = 0.0
    C["na_cmask"] = cm
    return C


_CACHE = {}


def kernel(**inputs):
    x = np.ascontiguousarray(inputs["x"], dtype=np.float32)
    if "nc" not in _CACHE:
        _CACHE["nc"] = Builder().build()
        _CACHE["consts"] = _consts()
    nc = _CACHE["nc"]
    C = _CACHE["consts"]
    f = lambda k: np.ascontiguousarray(inputs[k], dtype=np.float32)
    shared = {
        "ada_w": f("ada_w"), "ada_b": f("ada_b"), "norm1_g": f("norm1_g"), "norm2_g": f("norm2_g"),
        "w_in": f("w_in"), "w_out": f("w_out"), "ret_decay_logit": f("ret_decay_logit"), "ret_gn_g": f("ret_gn_g"),
        "conv_w": f("conv_w"), "na_rpb": f("na_rpb"), "ffn_w_in": f("ffn_w_in"), "ffn_w_out": f("ffn_w_out"),
        "moe_router_w": f("moe_router_w"), "moe_router_b": f("moe_router_b"), "moe_w_in": f("moe_w_in"),
        "moe_w_out": f("moe_w_out"), "final_g": f("final_g"),
    }
    shared.update(C)
    c = f("c"); ctx = f("ctx"); c_ctx = f("c_ctx")
    in_maps = []
    for core in range(8):
        b, half = core // 2, core % 2
        m = dict(shared)
        m["x"] = x[b]
        m["ctx"] = ctx[b]
        m["cc"] = np.stack([c[b], c_ctx], 0)
        sel = np.zeros((128, 2), np.float32)
        sel[:, half] = 1.0
        m["sel"] = sel
        in_maps.append(m)
    res = run_bass_kernel_spmd(nc, in_maps, core_ids=list(range(8)))
    out = np.empty((4, SEQ, D), np.float32)
    for core in range(8):
        b, half = core // 2, core % 2
        out[b, half * 2048:(half + 1) * 2048] = res.results[core]["out"]
    return out
```
